# Optimizing a Trainium2 kernel written in Bass

```python
import jax, jax.numpy as jnp
from jax import lax
import numpy as np

D_MODEL = 1024
BATCH = 8
SEQ = 4096
DEPTH = 2

N_HEADS = 16
HEAD_DIM = D_MODEL // N_HEADS
N_MIXERS = 2
MOBA_BLOCK = 256
MOBA_TOPK = 3
MOBA_Q_CHUNK = 64
SB_Q_BLOCK = 128
NORM_EPS = 1e-6

kernel_name = "moba_stickbreaking_interleaved_hybrid"


def rms_norm(x, g):
    xf = x.astype(jnp.float32)
    y = xf * lax.rsqrt(jnp.mean(xf * xf, axis=-1, keepdims=True) + NORM_EPS)
    return (y * g.astype(jnp.float32)).astype(x.dtype)


def alibi_slopes(n_heads):
    return jnp.asarray(2.0 ** (-8.0 * np.arange(1, n_heads + 1) / n_heads), dtype=jnp.float32)


def moba_attention(q, k, v):
    b, h, s, dh = q.shape
    n_blk = -(-s // MOBA_BLOCK)
    s_pad = n_blk * MOBA_BLOCK
    pad = ((0, 0), (0, 0), (0, s_pad - s), (0, 0))
    qp, kp, vp = jnp.pad(q, pad), jnp.pad(k, pad), jnp.pad(v, pad)
    k_blk = kp.reshape(b, h, n_blk, MOBA_BLOCK, dh)
    v_blk = vp.reshape(b, h, n_blk, MOBA_BLOCK, dh)
    k_mean = jnp.mean(k_blk.astype(jnp.float32), axis=3)
    n_sel = min(MOBA_TOPK, n_blk - 1)
    n_chunks = s_pad // MOBA_Q_CHUNK
    slopes = alibi_slopes(h)
    scale = dh ** -0.5
    blk_ar = jnp.arange(MOBA_BLOCK)

    def one_chunk(idx):
        bi = idx // n_chunks
        q0 = (idx % n_chunks) * MOBA_Q_CHUNK
        qf = lax.dynamic_slice_in_dim(qp[bi], q0, MOBA_Q_CHUNK, axis=1).astype(jnp.float32)
        cur = q0 // MOBA_BLOCK
        t_pos = q0 + jnp.arange(MOBA_Q_CHUNK)
        kb, vb = k_blk[bi], v_blk[bi]
        own_k = lax.dynamic_index_in_dim(kb, cur, axis=1, keepdims=False).astype(jnp.float32)
        own_v = lax.dynamic_index_in_dim(vb, cur, axis=1, keepdims=False).astype(jnp.float32)
        s_own = cur * MOBA_BLOCK + blk_ar
        dist_own = (t_pos[:, None] - s_own[None, :]).astype(jnp.float32)
        logit_own = jnp.einsum('hqd,hkd->hqk', qf, own_k) * scale - slopes[:, None, None] * dist_own[None]
        logit_own = jnp.where((s_own[None, :] <= t_pos[:, None])[None], logit_own, -jnp.inf)
        if n_sel == 0:
            probs = jax.nn.softmax(logit_own, axis=-1)
            return jnp.einsum('hqk,hkd->hqd', probs, own_v)
        gate = jnp.einsum('hqd,hnd->hqn', qf, k_mean[bi])
        gate = jnp.where((jnp.arange(n_blk) < cur)[None, None, :], gate, -jnp.inf)
        _, sel = lax.top_k(gate, n_sel)
        sel_valid = sel < cur
        k_sel = jax.vmap(lambda kh, ih: kh[ih])(kb, sel).astype(jnp.float32)
        v_sel = jax.vmap(lambda vh, ih: vh[ih])(vb, sel).astype(jnp.float32)
        s_sel = sel[..., None] * MOBA_BLOCK + blk_ar
        dist_sel = (t_pos[None, :, None, None] - s_sel).astype(jnp.float32)
        logit_sel = jnp.einsum('hqd,hqnkd->hqnk', qf, k_sel) * scale - slopes[:, None, None, None] * dist_sel
        logit_sel = jnp.where(sel_valid[..., None], logit_sel, -jnp.inf)
        n_past = n_sel * MOBA_BLOCK
        logits = jnp.concatenate([logit_sel.reshape(h, MOBA_Q_CHUNK, n_past), logit_own], axis=-1)
        probs = jax.nn.softmax(logits, axis=-1)
        p_sel = probs[..., :n_past].reshape(h, MOBA_Q_CHUNK, n_sel, MOBA_BLOCK)
        p_own = probs[..., n_past:]
        return (jnp.einsum('hqnk,hqnkd->hqd', p_sel, v_sel)
                + jnp.einsum('hqk,hkd->hqd', p_own, own_v))

    outs = lax.map(one_chunk, jnp.arange(b * n_chunks))
    outs = outs.reshape(b, n_chunks, h, MOBA_Q_CHUNK, dh).transpose(0, 2, 1, 3, 4)
    return outs.reshape(b, h, s_pad, dh)[:, :, :s].astype(q.dtype)


def stick_breaking_attention(q, k, v):
    b, h, s, dh = q.shape
    n_qb = s // SB_Q_BLOCK
    scale = dh ** -0.5
    s_pos = jnp.arange(s)

    def one_block(idx):
        bi = idx // n_qb
        q0 = (idx % n_qb) * SB_Q_BLOCK
        qf = lax.dynamic_slice_in_dim(q[bi], q0, SB_Q_BLOCK, axis=1).astype(jnp.float32)
        z = jnp.einsum('hqd,hkd->hqk', qf, k[bi].astype(jnp.float32)) * scale
        t_pos = q0 + jnp.arange(SB_Q_BLOCK)
        strict = (s_pos[None, :] < t_pos[:, None])[None]
        log_1m_beta = jnp.where(strict, jax.nn.log_sigmoid(-z), 0.0)
        suffix = lax.cumsum(log_1m_beta, axis=2, reverse=True) - log_1m_beta
        w = jnp.where(strict, jnp.exp(jax.nn.log_sigmoid(z) + suffix), 0.0)
        return jnp.einsum('hqk,hkd->hqd', w, v[bi].astype(jnp.float32))

    outs = lax.map(one_block, jnp.arange(b * n_qb))
    outs = outs.reshape(b, n_qb, h, SB_Q_BLOCK, dh).transpose(0, 2, 1, 3, 4)
    return outs.reshape(b, h, s, dh).astype(q.dtype)


def mixer_branch(hn, w_in, w_out, mixer_id):
    b, s, d = hn.shape
    proj = jnp.einsum('bsd,de->bse', hn, w_in)
    q, k, v, z = jnp.split(proj, 4, axis=-1)
    to_heads = lambda t: t.reshape(b, s, N_HEADS, HEAD_DIM).transpose(0, 2, 1, 3)
    if mixer_id == 0:
        o = moba_attention(to_heads(q), to_heads(k), to_heads(v))
    else:
        o = stick_breaking_attention(to_heads(q), to_heads(k), to_heads(v))
    o = o.transpose(0, 2, 1, 3).reshape(b, s, d)
    return jnp.einsum('bse,ed->bsd', o * jax.nn.silu(z), w_out)


def setup_inputs(seed: int = 0) -> dict:
    key = jax.random.key(seed)
    k_x, k_g, k_in, k_out, k_f = jax.random.split(key, 5)
    x = jax.random.normal(k_x, (BATCH, SEQ, D_MODEL), jnp.float32)
    norm_g = 1.0 + 0.02 * jax.random.normal(k_g, (DEPTH, D_MODEL), jnp.float32)
    w_in = jax.random.normal(k_in, (DEPTH, D_MODEL, 4 * D_MODEL), jnp.float32) * D_MODEL ** -0.5
    w_out = jax.random.normal(k_out, (DEPTH, D_MODEL, D_MODEL), jnp.float32) * D_MODEL ** -0.5
    final_g = 1.0 + 0.02 * jax.random.normal(k_f, (D_MODEL,), jnp.float32)
    return {"x": x, "norm_g": norm_g, "w_in": w_in, "w_out": w_out, "final_g": final_g}


def reference(x, norm_g, w_in, w_out, final_g):
    h = x
    for i in range(DEPTH):
        h = h + mixer_branch(rms_norm(h, norm_g[i]), w_in[i], w_out[i], i % N_MIXERS)
    return rms_norm(h, final_g)
```

```python
import contextlib
import numpy as np
import concourse.bass as bass
import concourse.mybir as mybir
from concourse.bass_utils import run_bass_kernel_spmd

F32 = mybir.dt.float32
BF16 = mybir.dt.bfloat16
AF = mybir.ActivationFunctionType
ALU = mybir.AluOpType
AX = mybir.AxisListType

S = 4096
D = 1024
H = 16
DH = 64
NT = S // 128
EPS = 1e-6
BIGPEN = 65536.0
SBPEN = 32768.0
SLOPES = [2.0 ** (-(h + 1) / 2.0) for h in range(H)]

ENGS = ("pe", "act", "dve", "pool", "sp")
ROT = 8000


class Op:
    __slots__ = ("eng", "fn", "deps", "signal", "seq", "semi", "is_dma", "key", "cnt", "idx")

    def __init__(self, eng, fn, is_dma=False, key=None):
        self.eng = eng
        self.fn = fn
        self.deps = []
        self.signal = False
        self.seq = 0
        self.semi = 0
        self.is_dma = is_dma
        self.key = key
        self.cnt = 0


class Sched:
    def __init__(self):
        self.ops = {e: [] for e in ENGS}
        self.lastw = {}
        self.readers = {}
        self.dma_cnt = {}
        self.all_dma_last = {}

    def add(self, eng, fn, reads=(), writes=(), dma_key=None):
        op = Op(eng, fn, is_dma=dma_key is not None, key=dma_key)
        deps = {}

        def dep(o, kind):
            if o is None or o is op:
                return
            if not o.is_dma and not op.is_dma and o.eng == eng:
                if eng == "pe":
                    return
            deps[id(o)] = o

        rset = []
        wset = list(writes)
        for r in reads:
            if r.startswith("ps"):
                wset.append(r)
            else:
                rset.append(r)
        for r in rset:
            dep(self.lastw.get(r), "raw")
        for w in wset:
            dep(self.lastw.get(w), "raw" if w.startswith("ps") else "waw")
            for o in self.readers.get(w, {}).values():
                dep(o, "war")
        op.deps = list(deps.values())
        for o in op.deps:
            o.signal = True
        for r in rset:
            d = self.readers.setdefault(r, {})
            d[("dma", id(op)) if op.is_dma else eng] = op
        for w in wset:
            self.lastw[w] = op
            self.readers[w] = {}
        if op.is_dma:
            self.dma_cnt[dma_key] = self.dma_cnt.get(dma_key, 0) + 1
            op.cnt = self.dma_cnt[dma_key]
            self.all_dma_last[dma_key] = op
        self.ops[eng].append(op)
        return op

    def barrier(self):
        lasts = []
        for e in ENGS:
            for o in reversed(self.ops[e]):
                if not o.is_dma and o.fn is not None:
                    lasts.append(o)
                    break
        lasts += list(self.all_dma_last.values())
        for e in ENGS:
            op = Op(e, None)
            op.deps = [o for o in lasts if o.is_dma or o.eng != e]
            for o in op.deps:
                o.signal = True
            self.ops[e].append(op)

    def finalize(self):
        self.nsem = {}
        for e in ENGS:
            c = 0
            for o in self.ops[e]:
                if o.signal and not o.is_dma:
                    o.semi = c // ROT
                    o.seq = c % ROT + 1
                    c += 1
            self.nsem[e] = max(1, (c + ROT - 1) // ROT)

    def emit(self, eng, e, esems, dsems):
        waited = {}
        for o in self.ops[eng]:
            for d in o.deps:
                if d.is_dma:
                    k = ("d", d.key)
                    v = 16 * d.cnt
                    sem = dsems[d.key]
                else:
                    k = (d.eng, d.semi)
                    v = d.seq
                    sem = esems[d.eng][d.semi]
                if waited.get(k, 0) >= v:
                    continue
                waited[k] = v
                e.wait_ge(sem, v)
            if o.fn is None:
                continue
            ins = o.fn(e)
            if o.is_dma:
                ins.then_inc(dsems[o.key], 16)
            elif o.signal:
                ins.then_inc(esems[eng][o.semi], 1)


def build_program(layers=(0, 1), debug=False, heads=None, stop=None):
    heads = list(range(H)) if heads is None else list(heads)
    nc = bass.Bass("TRN2", target_bir_lowering=False)
    x_d = nc.dram_tensor("x", [S, D], F32, kind="ExternalInput").ap()
    win_d = nc.dram_tensor("win", [2, H, 128, 8, 256], F32, kind="ExternalInput").ap()
    wout_d = nc.dram_tensor("wout", [2, 128, 8, D], F32, kind="ExternalInput").ap()
    gb_d = nc.dram_tensor("gb", [3, 128, D], F32, kind="ExternalInput").ap()
    cbf_d = nc.dram_tensor("cbf", [128, 512], F32, kind="ExternalInput").ap()
    cf_d = nc.dram_tensor("cf", [128, 512], F32, kind="ExternalInput").ap()
    augk_d = nc.dram_tensor("augk", [32, S], F32, kind="ExternalInput").ap()
    augq_d = nc.dram_tensor("augq", [16, S], F32, kind="ExternalInput").ap()
    out_d = nc.dram_tensor("out", [S, D], F32, kind="ExternalOutput").ap()
    if debug:
        h1_d = nc.dram_tensor("h1", [S, D], F32, kind="ExternalOutput").ap()
        gd_d = nc.dram_tensor("gd", [128, NT, D], BF16, kind="ExternalOutput").ap()
    else:
        h1_d = nc.dram_tensor("h1", [S, D], F32, kind="Internal").ap()

    sc = Sched()
    st = contextlib.ExitStack()
    with st:
        def sb(name, shape, dt):
            return st.enter_context(nc.sbuf_tensor("sb_" + name, shape, dt))

        hnT = sb("hnT", [128, 8, S], BF16)
        G = sb("G", [128, NT, D], BF16)
        cbf = sb("cbf", [128, 512], BF16)
        cf = sb("cf", [128, 512], F32)
        zeros = sb("zeros", [128, 512], F32)
        penpad = sb("penpad", [128, 4, 128], BF16)
        small = sb("small", [128, 64], F32)
        kms = sb("kms", [128, 16], F32)
        kmb = sb("kmb", [128, 16], BF16)
        gsb = sb("gsb", [128, 64], F32)
        m8 = sb("m8", [128, 32], F32)
        rec = sb("rec", [128, 8], F32)
        POOLW = 18296
        pool = sb("pool", [128, POOLW], F32)
        off = [0]

        def carve(words, dt=F32):
            a = off[0]
            off[0] += words
            assert off[0] <= POOLW, off[0]
            v = pool[:, a:a + words]
            if dt is BF16:
                v = v.bitcast(BF16)
            return v

        qkb = [carve(4096, BF16) for _ in range(2)]
        qTs = [b[:, 0:S] for b in qkb]
        kTs = [b[:, S:2 * S] for b in qkb]
        wh = [carve(1024, BF16).rearrange("p (k c) -> p k c", k=8) for _ in range(2)]
        V = carve(NT * 36, BF16).rearrange("p (t c) -> p t c", t=NT)
        sz = carve(NT * 64, F32).rearrange("p (t c) -> p t c", t=NT)
        wk0 = off[0]
        wTm = [carve(256, BF16) for _ in range(3)]
        off[0] = wk0
        abuf = [carve(512, F32) for _ in range(3)]
        incl = [carve(516, F32) for _ in range(3)]
        wbuf = [carve(256, BF16) for _ in range(3)]
        wTs = [carve(256, BF16) for _ in range(3)]
        endB = off[0]
        off[0] = 0
        wo = carve(4096, BF16).rearrange("p (k c) -> p k c", k=8)
        xt = [carve(1024, F32) for _ in range(4)]
        hn = [carve(512, BF16) for _ in range(4)]
        gbc = carve(1024, F32)
        GT = [carve(512, BF16).rearrange("p (k c) -> p k c", k=8) for _ in range(2)]
        junk = carve(512, BF16)
        assert off[0] <= POOLW

        ps = [st.enter_context(nc.psum_tensor(f"ps{i}", [128, 512], F32)) for i in range(8)]

        ident = cbf[:, 0:128]
        Jm = cbf[:, 128:256]
        tripen = cbf[:, 256:384]
        sbpen = cbf[:, 384:512]
        ss = small[:, 16:20]
        lnv = small[:, 20:24]
        rstd = small[:, 24:28]

        sc.add("pool", lambda e: e.dma_start(out=cbf[:], in_=cbf_d[:, :]), writes=["cbf"], dma_key="cst")
        sc.add("sp", lambda e: e.dma_start(out=cf[:], in_=cf_d[:, :]), writes=["cf"], dma_key="cst2")
        sc.add("pool", lambda e: e.memset(zeros[:], 0.0), writes=["zeros"])
        if debug:
            for t in range(NT):
                sc.add("pool", lambda e, t=t: e.memset(G[:, t, :], 0.0), writes=[f"G{t}"])
        sc.add("pool", lambda e: e.memset(penpad[:], 0.0), writes=["penpad"])

        def run_pipeline(items, stages, fillers=(), post=None):
            n = len(items)
            maxoff = max(o for _, o in stages)
            nsteps = n + maxoff
            fillers = list(fillers)
            nf = len(fillers)
            lo, hi = 8, max(9, nsteps - 8)
            fpos = {}
            for fi in range(nf):
                fpos.setdefault(lo + (fi * (hi - lo)) // max(nf, 1), []).append(fillers[fi])
            for i in range(nsteps):
                for fn, offk in stages:
                    j = i - offk
                    if 0 <= j < n:
                        fn(items[j])
                for f in fpos.get(i, ()):
                    f()
                if post:
                    for f in post.get(i - maxoff, ()):
                        f()

        def load_gb(i):
            sc.add("sp", lambda e: e.dma_start(out=gbc, in_=gb_d[i]), writes=["gbc"], dma_key="gbc")

        def hres(t0, n=1):
            return [f"hnT{t}" for t in range(t0, t0 + n)]

        def norm_part(s, final=False):
            sc.add("act", lambda e: e.activation(out=junk, in_=xt[s], func=AF.Square, accum_out=ss[:, s:s + 1]),
                   reads=[f"xt{s}"], writes=["junk", f"ss{s}"])
            sc.add("act", lambda e: e.activation(out=lnv[:, s:s + 1], in_=ss[:, s:s + 1], func=AF.Ln,
                                                 scale=1.0 / D, bias=eps_ap),
                   reads=[f"ss{s}", "eps"], writes=[f"lnv{s}"])
            sc.add("act", lambda e: e.activation(out=rstd[:, s:s + 1], in_=lnv[:, s:s + 1], func=AF.Exp, scale=-0.5),
                   reads=[f"lnv{s}"], writes=[f"rstd{s}"])
            if final:
                sc.add("dve", lambda e: e.scalar_tensor_tensor(out=xt[s], in0=xt[s], scalar=rstd[:, s:s + 1], in1=gbc,
                                                               op0=ALU.mult, op1=ALU.mult),
                       reads=[f"xt{s}", f"rstd{s}", "gbc"], writes=[f"xt{s}"])
            else:
                sc.add("dve", lambda e: e.scalar_tensor_tensor(out=hn[s], in0=xt[s], scalar=rstd[:, s:s + 1], in1=gbc,
                                                               op0=ALU.mult, op1=ALU.mult),
                       reads=[f"xt{s}", f"rstd{s}", "gbc"], writes=[f"hn{s}"])

        def tr_part(s, dst_tile, rev, psb):
            mat = Jm if rev else ident

            def tr(e, half):
                ins = None
                for c in range(4):
                    kc = half * 4 + c
                    ins = e.matmul(ps[psb + half][:, c * 128:(c + 1) * 128], lhsT=hn[s][:, kc * 128:(kc + 1) * 128],
                                   rhs=mat, start=True, stop=True)
                return ins
            for half in range(2):
                sc.add("pe", lambda e, half=half: tr(e, half), reads=[f"hn{s}", "cbf"], writes=[f"ps{psb + half}"])
            cols = slice(dst_tile * 128, (dst_tile + 1) * 128)
            sc.add("act", lambda e: e.activation(out=hnT[:, 0:4, cols], in_=ps[psb][:].rearrange("p (k c) -> p k c", k=4),
                                                 func=AF.Copy),
                   reads=[f"ps{psb}"], writes=hres(dst_tile))
            sc.add("dve", lambda e: e.tensor_copy(out=hnT[:, 4:8, cols],
                                                  in_=ps[psb + 1][:].rearrange("p (k c) -> p k c", k=4)),
                   reads=[f"ps{psb + 1}"], writes=hres(dst_tile))

        eps_ap = small[:, 8:9]
        ones_col = small[:, 9:10]
        sc.add("dve", lambda e: e.memset(eps_ap, EPS), writes=["eps"])
        sc.add("dve", lambda e: e.memset(ones_col, 1.0), writes=["ones"])

        if 0 in layers:
            load_gb(0)
            def a1(t):
                s_ = t % 4
                sc.add("sp", lambda e: e.dma_start(out=xt[s_], in_=x_d[t * 128:(t + 1) * 128, :]),
                       writes=[f"xt{s_}"], dma_key=f"xt{s_}")
                norm_part(s_)

            def a2(t):
                tr_part(t % 4, t, False, (t % 2) * 2)
            run_pipeline(list(range(NT)), [(a1, 0), (a2, 1)])
            sc.barrier()
            for par in range(2):
                sc.add("pool", lambda e, par=par: e.dma_start(out=kTs[par][64:96, :], in_=augk_d[:, :],
                                                              max_dma_last_dim=4096),
                       writes=[f"kTaug{par}"], dma_key=f"cstk{par}")
                sc.add("pool", lambda e, par=par: e.dma_start(out=qTs[par][80:96, :], in_=augq_d[:, :],
                                                              max_dma_last_dim=4096),
                       writes=[f"qTaug{par}"], dma_key=f"cstq{par}")
            sc.add("dve", lambda e: e.memset(V[:, :, 64:65], 1.0), writes=["Vones"])

        def load_wh(l, h, sl):
            sc.add("pool", lambda e: e.dma_start(out=wh[sl], in_=win_d[l, h], max_dma_last_dim=4096),
                   writes=[f"wh{sl}"], dma_key=f"wh{sl}")

        def qk_groups(l, h, sl, moba, par):
            w = wh[sl]
            qT, kT = qTs[par], kTs[par]
            qscale = (0.125 / SLOPES[h]) if moba else 0.125
            groups = []
            for which in range(2):
                for tt in range(8):
                    def grp(which=which, tt=tt):
                        bank = 6 + (tt % 2)

                        def mm(e):
                            ins = None
                            for kc in range(8):
                                ins = e.matmul(ps[bank][0:64, :], lhsT=w[:, kc, which * 64:(which + 1) * 64],
                                               rhs=hnT[:, kc, tt * 512:(tt + 1) * 512], start=(kc == 0),
                                               stop=(kc == 7))
                            return ins
                        sc.add("pe", mm, reads=[f"wh{sl}"] + hres(tt * 4, 4), writes=[f"ps{bank}"])
                        cols = slice(tt * 512, (tt + 1) * 512)
                        if which == 0:
                            if moba:
                                sc.add("dve", lambda e: e.tensor_scalar(out=qT[0:64, cols], in0=ps[bank][0:64, :],
                                                                        scalar1=qscale, scalar2=None, op0=ALU.mult),
                                       reads=[f"ps{bank}"], writes=[f"qT{par}"])
                            else:
                                sc.add("act", lambda e: e.activation(out=qT[0:64, cols], in_=ps[bank][0:64, :],
                                                                     func=AF.Copy, scale=qscale),
                                       reads=[f"ps{bank}"], writes=[f"qT{par}"])
                        else:
                            if moba:
                                sc.add("dve", lambda e: e.tensor_reduce(
                                    out=kms[0:64, 2 * tt:2 * tt + 2],
                                    in_=ps[bank][0:64, :].rearrange("p (a b) -> p a b", a=2), axis=AX.X, op=ALU.add),
                                    reads=[f"ps{bank}"], writes=["kms"])
                                sc.add("dve", lambda e: e.tensor_copy(out=kT[0:64, cols], in_=ps[bank][0:64, :]),
                                       reads=[f"ps{bank}"], writes=[f"kT{par}"])
                            else:
                                sc.add("act", lambda e: e.activation(out=kT[0:64, cols], in_=ps[bank][0:64, :],
                                                                     func=AF.Copy),
                                       reads=[f"ps{bank}"], writes=[f"kT{par}"])
                    groups.append(grp)
            if moba:
                def g0():
                    sc.add("dve", lambda e: e.tensor_scalar(out=kmb[0:64, :], in0=kms[0:64, :], scalar1=1.0 / 256.0,
                                                            scalar2=None, op0=ALU.mult),
                           reads=["kms"], writes=["kmb"])
                groups.append(g0)
                for g in range(8):
                    def ggrp(g=g):
                        def gmm(e):
                            ins = None
                            for i in range(4):
                                t = 4 * g + i
                                ins = e.matmul(ps[2][:, i * 16:(i + 1) * 16], lhsT=qT[0:64, t * 128:(t + 1) * 128],
                                               rhs=kmb[0:64, :], start=True, stop=True)
                            return ins
                        sc.add("pe", gmm, reads=[f"qT{par}", "kmb"], writes=["ps2"])
                        for i in range(4):
                            t = 4 * g + i
                            cur = t // 2
                            gs = gsb[:, i * 16:(i + 1) * 16]
                            sc.add("dve", lambda e, gs=gs: e.memset(gs, -3.0e38), writes=[f"gsb{i}"])
                            if cur > 0:
                                sc.add("dve", lambda e, i=i, cur=cur, gs=gs: e.tensor_copy(
                                    out=gs[:, 0:cur], in_=ps[2][:, i * 16:i * 16 + cur]),
                                    reads=["ps2"], writes=[f"gsb{i}"])
                            sc.add("dve", lambda e, i=i, gs=gs: e.max(out=m8[:, i * 8:(i + 1) * 8], in_=gs),
                                   reads=[f"gsb{i}"], writes=[f"m8{i}"])
                            sc.add("dve", lambda e, i=i, gs=gs: e.tensor_scalar(
                                out=penpad[:, i, 64:80], in0=gs, scalar1=m8[:, i * 8 + 2:i * 8 + 3],
                                scalar2=-BIGPEN, op0=ALU.is_lt, op1=ALU.mult),
                                reads=[f"gsb{i}", f"m8{i}"], writes=[f"penpad{i}"])
                            sc.add("dve", lambda e, i=i, cur=cur: e.memset(penpad[:, i, 64 + cur:65 + cur], 0.0),
                                   reads=[f"penpad{i}"], writes=[f"penpad{i}"])

                        def pmm(e):
                            ins = None
                            for i in range(4):
                                ins = e.matmul(ps[3][:, i * 128:(i + 1) * 128], lhsT=penpad[:, i, :], rhs=ident,
                                               start=True, stop=True)
                            return ins
                        sc.add("pe", pmm, reads=[f"penpad{i}" for i in range(4)] + ["penpad", "cbf"], writes=["ps3"])
                        sc.add("act", lambda e: e.activation(out=qT[64:80, g * 512:(g + 1) * 512], in_=ps[3][64:80, :],
                                                             func=AF.Copy),
                               reads=["ps3"], writes=[f"qT{par}"])
                    groups.append(ggrp)
            return groups

        def vz_groups(l, h, sl):
            w = wh[sl]
            groups = []
            for g in range(8):
                def grp(g=g):
                    bank = 6 + (g % 2)

                    def mmvz(e):
                        ins = None
                        for i in range(4):
                            t = 4 * g + i
                            for kc in range(8):
                                ins = e.matmul(ps[bank][:, i * 128:(i + 1) * 128],
                                               lhsT=hnT[:, kc, t * 128:(t + 1) * 128],
                                               rhs=w[:, kc, 128:256], start=(kc == 0), stop=(kc == 7))
                        return ins
                    sc.add("pe", mmvz, reads=[f"wh{sl}"] + hres(4 * g, 4), writes=[f"ps{bank}"])
                    pv = ps[bank][:].rearrange("p (i c) -> p i c", i=4)
                    sc.add("dve", lambda e: e.tensor_copy(out=V[:, 4 * g:4 * g + 4, 0:64], in_=pv[:, :, 0:64]),
                           reads=[f"ps{bank}"], writes=[f"V{4 * g + i}" for i in range(4)])
                    sc.add("act", lambda e: e.activation(out=sz[:, 4 * g:4 * g + 4, :], in_=pv[:, :, 64:128],
                                                         func=AF.Silu),
                           reads=[f"ps{bank}"], writes=[f"sz{4 * g + i}" for i in range(4)])
                groups.append(grp)
            return groups

        def moba_attn(h, par, fillers, vzg):
            slope = SLOPES[h]
            qT, kT = qTs[par], kTs[par]
            items = []
            post = {}
            for qt in range(7, -1, -1):
                nk = 4 * qt + 4
                for kt in range(nk):
                    items.append(dict(qt=qt, kt=kt, nk=nk, idx=len(items)))
                if vzg:
                    post[len(items) - 1] = [vzg[qt]]

            def stA(it):
                qt, kt, idx = it["qt"], it["kt"], it["idx"]
                j = kt - 4 * qt
                c0 = 128 * max(j, 0)
                sbk = idx % 2
                w_i = idx % 3

                def smm(e):
                    ins = e.matmul(ps[sbk][:, c0:512], lhsT=kT[0:82, kt * 128:(kt + 1) * 128],
                                   rhs=qT[0:82, qt * 512 + c0:(qt + 1) * 512], start=True, stop=(j < 0))
                    if j >= 0:
                        ins = e.matmul(ps[sbk][:, c0:c0 + 128], lhsT=ident, rhs=tripen, start=False, stop=True)
                    return ins
                sc.add("pe", smm, reads=[f"qT{par}", f"kT{par}", f"qTaug{par}", f"kTaug{par}", "cbf"],
                       writes=[f"ps{sbk}"])
                bcol = h * 32 + (j + 28)
                sc.add("act", lambda e: e.activation(out=wTm[w_i][:, c0:512], in_=ps[sbk][:, c0:512], func=AF.Exp,
                                                     scale=slope, bias=cf[:, bcol:bcol + 1]),
                       reads=[f"ps{sbk}", "cf"], writes=[f"wTm{w_i}"])

            def stB(it):
                qt, kt, nk, idx = it["qt"], it["kt"], it["nk"], it["idx"]
                j = kt - 4 * qt
                w_i = idx % 3
                ab = 4 + (qt % 2)

                def pvmm(e):
                    ins = None
                    for i in range(max(j, 0), 4):
                        ins = e.matmul(ps[ab][:, i * 128:i * 128 + 65], lhsT=wTm[w_i][:, i * 128:(i + 1) * 128],
                                       rhs=V[:, kt, 0:65], start=(kt == 0 and i == 0), stop=(kt == nk - 1),
                                       skip_group_check=True)
                    return ins
                sc.add("pe", pvmm, reads=[f"wTm{w_i}", f"V{kt}", "Vones"], writes=[f"ps{ab}"])
                if kt == nk - 1:
                    accv = ps[ab][:].rearrange("p (i c) -> p i c", i=4)
                    r0 = 4 * (qt % 2)
                    sc.add("dve", lambda e: e.reciprocal(
                        out=rec[:, r0:r0 + 4].rearrange("p (i o) -> p i o", o=1), in_=accv[:, :, 64:65]),
                        reads=[f"ps{ab}"], writes=[f"rec{qt % 2}"])
                    for i in range(4):
                        t = 4 * qt + i
                        sc.add("dve", lambda e, i=i, t=t: e.scalar_tensor_tensor(
                            out=G[:, t, h * 64:(h + 1) * 64], in0=accv[:, i, 0:64], scalar=rec[:, r0 + i:r0 + i + 1],
                            in1=sz[:, t, :], op0=ALU.mult, op1=ALU.mult),
                            reads=[f"ps{ab}", f"rec{qt % 2}", f"sz{t}"], writes=[f"G{t}"])
            run_pipeline(items, [(stA, 0), (stB, 2)], fillers, post)

        def sb_attn(h, par, fillers, vzg):
            qT, kT = qTs[par], kTs[par]
            items = []
            post = {}
            for iq in range(NT):
                nch = (NT - iq + 3) // 4
                for c in range(nch):
                    k0 = 128 * iq + 512 * c
                    N = min(512, S - k0)
                    items.append(dict(iq=iq, c=c, nch=nch, k0=k0, N=N, idx=len(items)))
                if vzg and iq % 4 == 3:
                    post[len(items) - 1] = [vzg[iq // 4]]

            def stA(it):
                iq, c, k0, N, idx = it["iq"], it["c"], it["k0"], it["N"], it["idx"]
                y = idx % 3
                sbk = idx % 2

                def smm(e):
                    ins = e.matmul(ps[sbk][:, 0:N], lhsT=qT[0:64, iq * 128:(iq + 1) * 128], rhs=kT[0:64, k0:k0 + N],
                                   start=True, stop=(c > 0))
                    if c == 0:
                        ins = e.matmul(ps[sbk][:, 0:128], lhsT=ident, rhs=sbpen, start=False, stop=True)
                    return ins
                sc.add("pe", smm, reads=[f"qT{par}", f"kT{par}", "cbf"], writes=[f"ps{sbk}"])
                sc.add("act", lambda e: e.activation(out=abuf[y][:, 0:N], in_=ps[sbk][:, 0:N], func=AF.Sigmoid,
                                                     scale=-1.0),
                       reads=[f"ps{sbk}"], writes=[f"abuf{y}"])

            def stA1(it):
                c, N, idx = it["c"], it["N"], it["idx"]
                y = idx % 3
                py = (idx - 1) % 3
                init = ones_col if c == 0 else incl[py][:, 512:513]
                rd = ["ones"] if c == 0 else [f"incl{py}"]
                sc.add("dve", lambda e: e.tensor_tensor_scan(
                    out=incl[y][:, 1:N + 1], data0=abuf[y][:, 0:N], data1=zeros[:, 0:N], initial=init,
                    op0=ALU.mult, op1=ALU.add),
                    reads=[f"abuf{y}", "zeros"] + rd, writes=[f"incl{y}"])

            def stB1(it):
                c, N, idx = it["c"], it["N"], it["idx"]
                y = idx % 3
                py = (idx - 1) % 3
                init = ones_col if c == 0 else incl[py][:, 512:513]
                rd = ["ones"] if c == 0 else [f"incl{py}"]
                sc.add("dve", lambda e: e.tensor_tensor(out=wbuf[y][:, 1:N], in0=incl[y][:, 1:N],
                                                        in1=incl[y][:, 2:N + 1], op=ALU.subtract),
                       reads=[f"incl{y}"], writes=[f"wbuf{y}"])
                sc.add("act", lambda e: e.activation(out=wbuf[y][:, 0:1], in_=incl[y][:, 1:2], func=AF.Identity,
                                                     scale=-1.0, bias=init),
                       reads=[f"incl{y}"] + rd, writes=[f"wbuf{y}"])

            def stT(it):
                N, idx = it["N"], it["idx"]
                y = idx % 3
                tbk = 2 + (idx % 2)
                nb = N // 128

                def tmm(e):
                    ins = None
                    for b in range(nb):
                        ins = e.matmul(ps[tbk][:, b * 128:(b + 1) * 128], lhsT=wbuf[y][:, b * 128:(b + 1) * 128],
                                       rhs=ident, start=True, stop=True)
                    return ins
                sc.add("pe", tmm, reads=[f"wbuf{y}", "cbf"], writes=[f"ps{tbk}"])

            def stE(it):
                N, idx = it["N"], it["idx"]
                y = idx % 3
                tbk = 2 + (idx % 2)
                sc.add("act", lambda e: e.activation(out=wTs[y][:, 0:N], in_=ps[tbk][:, 0:N], func=AF.Copy),
                       reads=[f"ps{tbk}"], writes=[f"wTs{y}"])

            def stC(it):
                iq, c, nch, N, idx = it["iq"], it["c"], it["nch"], it["N"], it["idx"]
                y = idx % 3
                nb = N // 128
                ab = 4 + (iq % 2)

                def pvmm(e):
                    ins = None
                    for b in range(nb):
                        ins = e.matmul(ps[ab][:, 0:64], lhsT=wTs[y][:, b * 128:(b + 1) * 128],
                                       rhs=V[:, iq + 4 * c + b, 0:64], start=(c == 0 and b == 0),
                                       stop=(c == nch - 1 and b == nb - 1))
                    return ins
                sc.add("pe", pvmm, reads=[f"wTs{y}"] + [f"V{iq + 4 * c + b}" for b in range(nb)], writes=[f"ps{ab}"])
                if c == nch - 1:
                    sc.add("dve", lambda e: e.tensor_tensor(out=G[:, iq, h * 64:(h + 1) * 64], in0=ps[ab][:, 0:64],
                                                            in1=sz[:, iq, :], op=ALU.mult),
                           reads=[f"ps{ab}", f"sz{iq}"], writes=[f"G{iq}"])
            run_pipeline(items, [(stA1, 1), (stE, 4), (stA, 0), (stC, 5), (stB1, 2), (stT, 3)], fillers, post)

        def phase_c(l, hsrc_d):
            sc.add("pool", lambda e: e.dma_start(out=wo, in_=wout_d[l], max_dma_last_dim=8192),
                   writes=["wo"], dma_key="wo")
            load_gb(l + 1)
            rev = (l == 1)
            mat = Jm if rev else ident

            def c1(m):
                nat = (NT - 1 - m) if rev else m
                s_ = m % 4
                g_ = m % 2
                sc.add("sp", lambda e: e.dma_start(out=xt[s_], in_=hsrc_d[nat * 128:(nat + 1) * 128, :]),
                       writes=[f"xt{s_}"], dma_key=f"xt{s_}")

                def tr(e, half):
                    ins = None
                    for c in range(4):
                        kc = half * 4 + c
                        ins = e.matmul(ps[half][:, c * 128:(c + 1) * 128], lhsT=G[:, m, kc * 128:(kc + 1) * 128],
                                       rhs=mat, start=True, stop=True)
                    return ins
                for half in range(2):
                    sc.add("pe", lambda e, half=half: tr(e, half), reads=[f"G{m}", "cbf"], writes=[f"ps{half}"])
                sc.add("act", lambda e: e.activation(out=GT[g_][:, 0:4, :],
                                                     in_=ps[0][:].rearrange("p (k c) -> p k c", k=4), func=AF.Copy),
                       reads=["ps0"], writes=[f"GT{g_}"])
                sc.add("dve", lambda e: e.tensor_copy(out=GT[g_][:, 4:8, :],
                                                      in_=ps[1][:].rearrange("p (k c) -> p k c", k=4)),
                       reads=["ps1"], writes=[f"GT{g_}"])

            def c2(m):
                s_ = m % 4
                g_ = m % 2
                for half in range(2):
                    def omm(e, half=half):
                        ins = None
                        for kc in range(8):
                            ins = e.matmul(ps[2 + half][:, :], lhsT=GT[g_][:, kc, :],
                                           rhs=wo[:, kc, half * 512:(half + 1) * 512], start=(kc == 0), stop=(kc == 7))
                        return ins
                    sc.add("pe", omm, reads=[f"GT{g_}", "wo"], writes=[f"ps{2 + half}"])
                    sc.add("dve", lambda e, half=half: e.tensor_tensor(
                        out=xt[s_][:, half * 512:(half + 1) * 512], in0=ps[2 + half][:, :],
                        in1=xt[s_][:, half * 512:(half + 1) * 512], op=ALU.add),
                        reads=[f"ps{2 + half}", f"xt{s_}"], writes=[f"xt{s_}"])

            def c3(m):
                nat = (NT - 1 - m) if rev else m
                s_ = m % 4
                if l == 0:
                    sc.add("sp", lambda e: e.dma_start(out=h1_d[nat * 128:(nat + 1) * 128, :], in_=xt[s_]),
                           reads=[f"xt{s_}"], writes=[f"h1_{nat}"], dma_key=f"st{s_}")
                    if 1 in layers:
                        norm_part(s_)
                else:
                    norm_part(s_, final=True)

            def c4(m):
                nat = (NT - 1 - m) if rev else m
                s_ = m % 4
                if l == 0:
                    if 1 in layers:
                        tr_part(s_, NT - 1 - nat, True, 4)
                else:
                    sc.add("sp", lambda e: e.dma_start(out=out_d[nat * 128:(nat + 1) * 128, :], in_=xt[s_]),
                           reads=[f"xt{s_}"], writes=[f"out_{nat}"], dma_key=f"st{s_}")
            run_pipeline(list(range(NT)), [(c1, 0), (c2, 1), (c3, 2), (c4, 3)])

        for l in layers:
            moba = (l == 0)
            if l == 1 and 0 not in layers:
                raise NotImplementedError
            load_wh(l, heads[0], 0)
            if stop != "A":
                for grp in qk_groups(l, heads[0], 0, moba, 0):
                    grp()
            if stop != "A":
                for grp in vz_groups(l, heads[0], 0):
                    grp()
            for hi, h in enumerate(heads):
                if stop == "A":
                    continue
                par = hi % 2
                fill = []
                vzg = None
                if hi + 1 < len(heads):
                    load_wh(l, heads[hi + 1], (hi + 1) % 2)
                    fill = qk_groups(l, heads[hi + 1], (hi + 1) % 2, moba, 1 - par)
                    vzg = vz_groups(l, heads[hi + 1], (hi + 1) % 2)
                if moba:
                    moba_attn(h, par, fill, vzg)
                else:
                    sb_attn(h, par, fill, vzg)
            sc.barrier()
            if debug and l == layers[-1]:
                sc.add("sp", lambda e: e.dma_start(out=gd_d[:, :, :], in_=G[:, :, :]), reads=[], writes=["gd"], dma_key="stg")
                sc.barrier()
            if stop in (None, "C"):
                phase_c(l, x_d if l == 0 else h1_d)
            sc.barrier()
        if 1 not in layers:
            pass

        fin = Op("sp", None)
        fin.deps = [o for k, o in sc.all_dma_last.items() if k.startswith("st")]
        sc.ops["sp"].append(fin)

        sc.finalize()
        esems = {e: [st.enter_context(nc.semaphore(f"s_{e}{i}")) for i in range(sc.nsem[e])] for e in ENGS}
        dsems = {k: st.enter_context(nc.semaphore(f"d_{k}")) for k in sc.dma_cnt}
        block = st.enter_context(nc.Block())

        @block.tensor
        def _(e):
            sc.emit("pe", e, esems, dsems)

        @block.scalar
        def _(e):
            sc.emit("act", e, esems, dsems)

        @block.vector
        def _(e):
            sc.emit("dve", e, esems, dsems)

        @block.gpsimd
        def _(e):
            sc.emit("pool", e, esems, dsems)

        @block.sync
        def _(e):
            sc.emit("sp", e, esems, dsems)
    return nc


def _consts():
    cbf = np.zeros((128, 512), np.float32)
    p = np.arange(128)
    cbf[p, p] = 1.0
    cbf[p, 128 + (127 - p)] = 1.0
    cbf[:, 256:384] = np.where(p[:, None] > p[None, :], -BIGPEN, 0.0)
    cbf[:, 384:512] = np.where(p[None, :] <= p[:, None], -SBPEN, 0.0)
    cf = np.zeros((128, 512), np.float32)
    for h in range(H):
        for idx in range(32):
            cf[:, h * 32 + idx] = np.float32(SLOPES[h]) * (128.0 * (idx - 28) + p)
    t = np.arange(S)
    augk = np.zeros((32, S), np.float32)
    augk[t // 256, t] = 1.0
    augk[16:18, :] = 1.0
    augq = np.zeros((16, S), np.float32)
    augq[0] = -128.0 * ((t % 512) // 128)
    augq[1] = -(t % 128)
    return cbf, cf, augk, augq


_NC_CACHE = {}


def kernel(x, norm_g, w_in, w_out, final_g):
    x = np.asarray(x, np.float32)
    w_in = np.asarray(w_in, np.float32)
    w_out = np.asarray(w_out, np.float32)
    norm_g = np.asarray(norm_g, np.float32)
    final_g = np.asarray(final_g, np.float32)
    B = x.shape[0]
    wi = w_in.reshape(2, 8, 128, 4, H, DH)
    win = np.ascontiguousarray(wi.transpose(0, 4, 2, 1, 3, 5)).reshape(2, H, 128, 8, 256)
    wout = np.ascontiguousarray(w_out.reshape(2, 8, 128, D).transpose(0, 2, 1, 3))
    gb = np.ascontiguousarray(np.broadcast_to(
        np.concatenate([norm_g, final_g[None, :]], axis=0)[:, None, :], (3, 128, D)))
    cbf, cf, augk, augq = _consts()
    if "nc" not in _NC_CACHE:
        _NC_CACHE["nc"] = build_program()
    nc = _NC_CACHE["nc"]
    in_maps = [{"x": np.ascontiguousarray(x[b]), "win": win, "wout": wout, "gb": gb, "cbf": cbf, "cf": cf,
                "augk": augk, "augq": augq} for b in range(B)]
    res = run_bass_kernel_spmd(nc, in_maps, core_ids=list(range(B)))
    return np.stack([np.asarray(r["out"]) for r in res.results], axis=0).astype(np.float32)
```

```python
import contextlib
import numpy as np
import concourse.bass as bass
import concourse.mybir as mybir
from concourse.bass_utils import run_bass_kernel_spmd

F32 = mybir.dt.float32
BF16 = mybir.dt.bfloat16
AF = mybir.ActivationFunctionType
ALU = mybir.AluOpType
AX = mybir.AxisListType

S = 4096
D = 1024
H = 16
DH = 64
NT = S // 128
EPS = 1e-6
BIGPEN = 65536.0
SBPEN = 32768.0
SLOPES = [2.0 ** (-(h + 1) / 2.0) for h in range(H)]

ENGS = ("pe", "act", "dve", "pool", "sp")
ROT = 8000


class Op:
    __slots__ = ("eng", "fn", "deps", "signal", "seq", "semi", "is_dma", "key", "cnt", "idx")

    def __init__(self, eng, fn, is_dma=False, key=None):
        self.eng = eng
        self.fn = fn
        self.deps = []
        self.signal = False
        self.seq = 0
        self.semi = 0
        self.is_dma = is_dma
        self.key = key
        self.cnt = 0


class Sched:
    def __init__(self):
        self.ops = {e: [] for e in ENGS}
        self.lastw = {}
        self.readers = {}
        self.dma_cnt = {}
        self.all_dma_last = {}

    def add(self, eng, fn, reads=(), writes=(), dma_key=None):
        op = Op(eng, fn, is_dma=dma_key is not None, key=dma_key)
        deps = {}

        def dep(o, kind):
            if o is None or o is op:
                return
            if not o.is_dma and not op.is_dma and o.eng == eng:
                if eng == "pe":
                    return
            deps[id(o)] = o

        rset = []
        wset = list(writes)
        for r in reads:
            if r.startswith("ps"):
                wset.append(r)
            else:
                rset.append(r)
        for r in rset:
            dep(self.lastw.get(r), "raw")
        for w in wset:
            dep(self.lastw.get(w), "raw" if w.startswith("ps") else "waw")
            for o in self.readers.get(w, {}).values():
                dep(o, "war")
        op.deps = list(deps.values())
        for o in op.deps:
            o.signal = True
        for r in rset:
            d = self.readers.setdefault(r, {})
            d[("dma", id(op)) if op.is_dma else eng] = op
        for w in wset:
            self.lastw[w] = op
            self.readers[w] = {}
        if op.is_dma:
            self.dma_cnt[dma_key] = self.dma_cnt.get(dma_key, 0) + 1
            op.cnt = self.dma_cnt[dma_key]
            self.all_dma_last[dma_key] = op
        self.ops[eng].append(op)
        return op

    def barrier(self):
        lasts = []
        for e in ENGS:
            for o in reversed(self.ops[e]):
                if not o.is_dma and o.fn is not None:
                    lasts.append(o)
                    break
        lasts += list(self.all_dma_last.values())
        for e in ENGS:
            op = Op(e, None)
            op.deps = [o for o in lasts if o.is_dma or o.eng != e]
            for o in op.deps:
                o.signal = True
            self.ops[e].append(op)

    def finalize(self):
        self.nsem = {}
        for e in ENGS:
            c = 0
            for o in self.ops[e]:
                if o.signal and not o.is_dma:
                    o.semi = c // ROT
                    o.seq = c % ROT + 1
                    c += 1
            self.nsem[e] = max(1, (c + ROT - 1) // ROT)

    def emit(self, eng, e, esems, dsems):
        waited = {}
        for o in self.ops[eng]:
            for d in o.deps:
                if d.is_dma:
                    k = ("d", d.key)
                    v = 16 * d.cnt
                    sem = dsems[d.key]
                else:
                    k = (d.eng, d.semi)
                    v = d.seq
                    sem = esems[d.eng][d.semi]
                if waited.get(k, 0) >= v:
                    continue
                waited[k] = v
                e.wait_ge(sem, v)
            if o.fn is None:
                continue
            ins = o.fn(e)
            if o.is_dma:
                ins.then_inc(dsems[o.key], 16)
            elif o.signal:
                ins.then_inc(esems[eng][o.semi], 1)


def build_program(layers=(0, 1), debug=False, heads=None, stop=None):
    heads = list(range(H)) if heads is None else list(heads)
    nc = bass.Bass("TRN2", target_bir_lowering=False)
    x_d = nc.dram_tensor("x", [S, D], F32, kind="ExternalInput").ap()
    win_d = nc.dram_tensor("win", [2, H, 128, 8, 256], F32, kind="ExternalInput").ap()
    wout_d = nc.dram_tensor("wout", [2, 128, 8, D], F32, kind="ExternalInput").ap()
    gb_d = nc.dram_tensor("gb", [3, 128, D], F32, kind="ExternalInput").ap()
    cbf_d = nc.dram_tensor("cbf", [128, 512], F32, kind="ExternalInput").ap()
    cf_d = nc.dram_tensor("cf", [128, 512], F32, kind="ExternalInput").ap()
    augk_d = nc.dram_tensor("augk", [32, S], F32, kind="ExternalInput").ap()
    augq_d = nc.dram_tensor("augq", [16, S], F32, kind="ExternalInput").ap()
    out_d = nc.dram_tensor("out", [S, D], F32, kind="ExternalOutput").ap()
    if debug:
        h1_d = nc.dram_tensor("h1", [S, D], F32, kind="ExternalOutput").ap()
        gd_d = nc.dram_tensor("gd", [128, NT, D], BF16, kind="ExternalOutput").ap()
    else:
        h1_d = nc.dram_tensor("h1", [S, D], F32, kind="Internal").ap()

    sc = Sched()
    st = contextlib.ExitStack()
    with st:
        def sb(name, shape, dt):
            return st.enter_context(nc.sbuf_tensor("sb_" + name, shape, dt))

        hnT = sb("hnT", [128, 8, S], BF16)
        G = sb("G", [128, NT, D], BF16)
        cbf = sb("cbf", [128, 512], BF16)
        cf = sb("cf", [128, 512], F32)
        zeros = sb("zeros", [128, 512], F32)
        penpad = sb("penpad", [128, 4, 128], BF16)
        small = sb("small", [128, 64], F32)
        kms = sb("kms", [128, 16], F32)
        kmb = sb("kmb", [128, 16], BF16)
        gsb = sb("gsb", [128, 64], F32)
        m8 = sb("m8", [128, 32], F32)
        rec = sb("rec", [128, 8], F32)
        POOLW = 18600
        pool = sb("pool", [128, POOLW], F32)
        off = [0]

        def carve(words, dt=F32):
            a = off[0]
            off[0] += words
            assert off[0] <= POOLW, off[0]
            v = pool[:, a:a + words]
            if dt is BF16:
                v = v.bitcast(BF16)
            return v

        qkb = [carve(4096, BF16) for _ in range(2)]
        qTs = [b[:, 0:S] for b in qkb]
        kTs = [b[:, S:2 * S] for b in qkb]
        wh = [carve(1024, BF16).rearrange("p (k c) -> p k c", k=8) for _ in range(2)]
        V = carve(NT * 36, BF16).rearrange("p (t c) -> p t c", t=NT)
        sz = carve(NT * 64, F32).rearrange("p (t c) -> p t c", t=NT)
        wk0 = off[0]
        wTm = [carve(256, BF16) for _ in range(3)]
        off[0] = wk0
        abuf = [carve(512, F32) for _ in range(3)]
        incl = [carve(516, F32) for _ in range(4)]
        wbuf = [carve(256, BF16) for _ in range(3)]
        wTs = [carve(256, BF16) for _ in range(3)]
        endB = off[0]
        off[0] = 0
        wo = carve(4096, BF16).rearrange("p (k c) -> p k c", k=8)
        xt = [carve(1024, F32) for _ in range(4)]
        hn = [carve(512, BF16) for _ in range(4)]
        gbc = carve(1024, F32)
        GT = [carve(512, BF16).rearrange("p (k c) -> p k c", k=8) for _ in range(2)]
        junk = carve(512, BF16)
        assert off[0] <= POOLW

        ps = [st.enter_context(nc.psum_tensor(f"ps{i}", [128, 512], F32)) for i in range(8)]

        ident = cbf[:, 0:128]
        Jm = cbf[:, 128:256]
        tripen = cbf[:, 256:384]
        sbpen = cbf[:, 384:512]
        ss = small[:, 16:20]
        lnv = small[:, 20:24]
        rstd = small[:, 24:28]

        sc.add("pool", lambda e: e.dma_start(out=cbf[:], in_=cbf_d[:, :]), writes=["cbf"], dma_key="cst")
        sc.add("sp", lambda e: e.dma_start(out=cf[:], in_=cf_d[:, :]), writes=["cf"], dma_key="cst2")
        sc.add("pool", lambda e: e.memset(zeros[:], 0.0), writes=["zeros"])
        if debug:
            for t in range(NT):
                sc.add("pool", lambda e, t=t: e.memset(G[:, t, :], 0.0), writes=[f"G{t}"])
        sc.add("pool", lambda e: e.memset(penpad[:], 0.0), writes=["penpad"])

        def run_pipeline(items, stages, fillers=(), post=None):
            n = len(items)
            maxoff = max(o for _, o in stages)
            nsteps = n + maxoff
            fillers = list(fillers)
            nf = len(fillers)
            lo, hi = 8, max(9, nsteps - 8)
            fpos = {}
            for fi in range(nf):
                fpos.setdefault(lo + (fi * (hi - lo)) // max(nf, 1), []).append(fillers[fi])
            for i in range(nsteps):
                for fn, offk in stages:
                    j = i - offk
                    if 0 <= j < n:
                        fn(items[j])
                for f in fpos.get(i, ()):
                    f()
                if post:
                    for f in post.get(i - maxoff, ()):
                        f()

        def load_gb(i):
            sc.add("sp", lambda e: e.dma_start(out=gbc, in_=gb_d[i]), writes=["gbc"], dma_key="gbc")

        def hres(t0, n=1):
            return [f"hnT{t}" for t in range(t0, t0 + n)]

        def norm_part(s, final=False):
            sc.add("act", lambda e: e.activation(out=junk, in_=xt[s], func=AF.Square, accum_out=ss[:, s:s + 1]),
                   reads=[f"xt{s}"], writes=["junk", f"ss{s}"])
            sc.add("act", lambda e: e.activation(out=lnv[:, s:s + 1], in_=ss[:, s:s + 1], func=AF.Ln,
                                                 scale=1.0 / D, bias=eps_ap),
                   reads=[f"ss{s}", "eps"], writes=[f"lnv{s}"])
            sc.add("act", lambda e: e.activation(out=rstd[:, s:s + 1], in_=lnv[:, s:s + 1], func=AF.Exp, scale=-0.5),
                   reads=[f"lnv{s}"], writes=[f"rstd{s}"])
            if final:
                sc.add("dve", lambda e: e.scalar_tensor_tensor(out=xt[s], in0=xt[s], scalar=rstd[:, s:s + 1], in1=gbc,
                                                               op0=ALU.mult, op1=ALU.mult),
                       reads=[f"xt{s}", f"rstd{s}", "gbc"], writes=[f"xt{s}"])
            else:
                sc.add("dve", lambda e: e.scalar_tensor_tensor(out=hn[s], in0=xt[s], scalar=rstd[:, s:s + 1], in1=gbc,
                                                               op0=ALU.mult, op1=ALU.mult),
                       reads=[f"xt{s}", f"rstd{s}", "gbc"], writes=[f"hn{s}"])

        def tr_part(s, dst_tile, rev, psb):
            mat = Jm if rev else ident

            def tr(e, half):
                ins = None
                for c in range(4):
                    kc = half * 4 + c
                    ins = e.matmul(ps[psb + half][:, c * 128:(c + 1) * 128], lhsT=hn[s][:, kc * 128:(kc + 1) * 128],
                                   rhs=mat, start=True, stop=True)
                return ins
            for half in range(2):
                sc.add("pe", lambda e, half=half: tr(e, half), reads=[f"hn{s}", "cbf"], writes=[f"ps{psb + half}"])
            cols = slice(dst_tile * 128, (dst_tile + 1) * 128)
            sc.add("act", lambda e: e.activation(out=hnT[:, 0:4, cols], in_=ps[psb][:].rearrange("p (k c) -> p k c", k=4),
                                                 func=AF.Copy),
                   reads=[f"ps{psb}"], writes=hres(dst_tile))
            sc.add("dve", lambda e: e.tensor_copy(out=hnT[:, 4:8, cols],
                                                  in_=ps[psb + 1][:].rearrange("p (k c) -> p k c", k=4)),
                   reads=[f"ps{psb + 1}"], writes=hres(dst_tile))

        eps_ap = small[:, 8:9]
        ones_col = small[:, 9:10]
        sc.add("dve", lambda e: e.memset(eps_ap, EPS), writes=["eps"])
        sc.add("dve", lambda e: e.memset(ones_col, 1.0), writes=["ones"])

        if 0 in layers:
            load_gb(0)
            def a1(t):
                s_ = t % 4
                sc.add("sp", lambda e: e.dma_start(out=xt[s_], in_=x_d[t * 128:(t + 1) * 128, :]),
                       writes=[f"xt{s_}"], dma_key=f"xt{s_}")
                norm_part(s_)

            def a2(t):
                tr_part(t % 4, t, False, (t % 2) * 2)
            run_pipeline(list(range(NT)), [(a1, 0), (a2, 1)])
            sc.barrier()
            for par in range(2):
                sc.add("pool", lambda e, par=par: e.dma_start(out=kTs[par][64:96, :], in_=augk_d[:, :],
                                                              max_dma_last_dim=4096),
                       writes=[f"kTaug{par}"], dma_key=f"cstk{par}")
                sc.add("pool", lambda e, par=par: e.dma_start(out=qTs[par][80:96, :], in_=augq_d[:, :],
                                                              max_dma_last_dim=4096),
                       writes=[f"qTaug{par}"], dma_key=f"cstq{par}")
            sc.add("dve", lambda e: e.memset(V[:, :, 64:65], 1.0), writes=["Vones"])

        def load_wh(l, h, sl):
            sc.add("pool", lambda e: e.dma_start(out=wh[sl], in_=win_d[l, h], max_dma_last_dim=4096),
                   writes=[f"wh{sl}"], dma_key=f"wh{sl}")

        def qk_groups(l, h, sl, moba, par):
            w = wh[sl]
            qT, kT = qTs[par], kTs[par]
            qscale = (0.125 / SLOPES[h]) if moba else 0.125
            groups = []
            for which in range(2):
                for tt in range(8):
                    def grp(which=which, tt=tt):
                        bank = 6 + (tt % 2)

                        def mm(e):
                            ins = None
                            for kc in range(8):
                                ins = e.matmul(ps[bank][0:64, :], lhsT=w[:, kc, which * 64:(which + 1) * 64],
                                               rhs=hnT[:, kc, tt * 512:(tt + 1) * 512], start=(kc == 0),
                                               stop=(kc == 7))
                            return ins
                        sc.add("pe", mm, reads=[f"wh{sl}"] + hres(tt * 4, 4), writes=[f"ps{bank}"])
                        cols = slice(tt * 512, (tt + 1) * 512)
                        if which == 0:
                            if moba:
                                sc.add("dve", lambda e: e.tensor_scalar(out=qT[0:64, cols], in0=ps[bank][0:64, :],
                                                                        scalar1=qscale, scalar2=None, op0=ALU.mult),
                                       reads=[f"ps{bank}"], writes=[f"qT{par}"])
                            else:
                                sc.add("act", lambda e: e.activation(out=qT[0:64, cols], in_=ps[bank][0:64, :],
                                                                     func=AF.Copy, scale=qscale),
                                       reads=[f"ps{bank}"], writes=[f"qT{par}"])
                        else:
                            if moba:
                                sc.add("dve", lambda e: e.tensor_reduce(
                                    out=kms[0:64, 2 * tt:2 * tt + 2],
                                    in_=ps[bank][0:64, :].rearrange("p (a b) -> p a b", a=2), axis=AX.X, op=ALU.add),
                                    reads=[f"ps{bank}"], writes=["kms"])
                                sc.add("dve", lambda e: e.tensor_copy(out=kT[0:64, cols], in_=ps[bank][0:64, :]),
                                       reads=[f"ps{bank}"], writes=[f"kT{par}"])
                            else:
                                sc.add("act", lambda e: e.activation(out=kT[0:64, cols], in_=ps[bank][0:64, :],
                                                                     func=AF.Copy),
                                       reads=[f"ps{bank}"], writes=[f"kT{par}"])
                    groups.append(grp)
            if moba:
                def g0():
                    sc.add("dve", lambda e: e.tensor_scalar(out=kmb[0:64, :], in0=kms[0:64, :], scalar1=1.0 / 256.0,
                                                            scalar2=None, op0=ALU.mult),
                           reads=["kms"], writes=["kmb"])
                groups.append(g0)
                for g in range(8):
                    def ggrp(g=g):
                        def gmm(e):
                            ins = None
                            for i in range(4):
                                t = 4 * g + i
                                ins = e.matmul(ps[2][:, i * 16:(i + 1) * 16], lhsT=qT[0:64, t * 128:(t + 1) * 128],
                                               rhs=kmb[0:64, :], start=True, stop=True)
                            return ins
                        sc.add("pe", gmm, reads=[f"qT{par}", "kmb"], writes=["ps2"])
                        for i in range(4):
                            t = 4 * g + i
                            cur = t // 2
                            gs = gsb[:, i * 16:(i + 1) * 16]
                            sc.add("dve", lambda e, gs=gs: e.memset(gs, -3.0e38), writes=[f"gsb{i}"])
                            if cur > 0:
                                sc.add("dve", lambda e, i=i, cur=cur, gs=gs: e.tensor_copy(
                                    out=gs[:, 0:cur], in_=ps[2][:, i * 16:i * 16 + cur]),
                                    reads=["ps2"], writes=[f"gsb{i}"])
                            sc.add("dve", lambda e, i=i, gs=gs: e.max(out=m8[:, i * 8:(i + 1) * 8], in_=gs),
                                   reads=[f"gsb{i}"], writes=[f"m8{i}"])
                            sc.add("dve", lambda e, i=i, gs=gs: e.tensor_scalar(
                                out=penpad[:, i, 64:80], in0=gs, scalar1=m8[:, i * 8 + 2:i * 8 + 3],
                                scalar2=-BIGPEN, op0=ALU.is_lt, op1=ALU.mult),
                                reads=[f"gsb{i}", f"m8{i}"], writes=[f"penpad{i}"])
                            sc.add("dve", lambda e, i=i, cur=cur: e.memset(penpad[:, i, 64 + cur:65 + cur], 0.0),
                                   reads=[f"penpad{i}"], writes=[f"penpad{i}"])

                        def pmm(e):
                            ins = None
                            for i in range(4):
                                ins = e.matmul(ps[3][:, i * 128:(i + 1) * 128], lhsT=penpad[:, i, :], rhs=ident,
                                               start=True, stop=True)
                            return ins
                        sc.add("pe", pmm, reads=[f"penpad{i}" for i in range(4)] + ["penpad", "cbf"], writes=["ps3"])
                        sc.add("act", lambda e: e.activation(out=qT[64:80, g * 512:(g + 1) * 512], in_=ps[3][64:80, :],
                                                             func=AF.Copy),
                               reads=["ps3"], writes=[f"qT{par}"])
                    groups.append(ggrp)
            return groups

        def vz_groups(l, h, sl):
            w = wh[sl]
            groups = []
            for g in range(8):
                def grp(g=g):
                    bank = 6 + (g % 2)

                    def mmvz(e):
                        ins = None
                        for i in range(4):
                            t = 4 * g + i
                            for kc in range(8):
                                ins = e.matmul(ps[bank][:, i * 128:(i + 1) * 128],
                                               lhsT=hnT[:, kc, t * 128:(t + 1) * 128],
                                               rhs=w[:, kc, 128:256], start=(kc == 0), stop=(kc == 7))
                        return ins
                    sc.add("pe", mmvz, reads=[f"wh{sl}"] + hres(4 * g, 4), writes=[f"ps{bank}"])
                    pv = ps[bank][:].rearrange("p (i c) -> p i c", i=4)
                    sc.add("dve", lambda e: e.tensor_copy(out=V[:, 4 * g:4 * g + 4, 0:64], in_=pv[:, :, 0:64]),
                           reads=[f"ps{bank}"], writes=[f"V{4 * g + i}" for i in range(4)])
                    sc.add("act", lambda e: e.activation(out=sz[:, 4 * g:4 * g + 4, :], in_=pv[:, :, 64:128],
                                                         func=AF.Silu),
                           reads=[f"ps{bank}"], writes=[f"sz{4 * g + i}" for i in range(4)])
                groups.append(grp)
            return groups

        def moba_attn(h, par, fillers, vzg):
            slope = SLOPES[h]
            qT, kT = qTs[par], kTs[par]
            items = []
            post = {}
            for qt in range(7, -1, -1):
                nk = 4 * qt + 4
                for kt in range(nk):
                    items.append(dict(qt=qt, kt=kt, nk=nk, idx=len(items)))
                if vzg:
                    post[len(items) - 1] = [vzg[qt]]

            def stA(it):
                qt, kt, idx = it["qt"], it["kt"], it["idx"]
                j = kt - 4 * qt
                c0 = 128 * max(j, 0)
                sbk = idx % 2
                w_i = idx % 3

                def smm(e):
                    ins = e.matmul(ps[sbk][:, c0:512], lhsT=kT[0:82, kt * 128:(kt + 1) * 128],
                                   rhs=qT[0:82, qt * 512 + c0:(qt + 1) * 512], start=True, stop=(j < 0))
                    if j >= 0:
                        ins = e.matmul(ps[sbk][:, c0:c0 + 128], lhsT=ident, rhs=tripen, start=False, stop=True)
                    return ins
                sc.add("pe", smm, reads=[f"qT{par}", f"kT{par}", f"qTaug{par}", f"kTaug{par}", "cbf"],
                       writes=[f"ps{sbk}"])
                bcol = h * 32 + (j + 28)
                sc.add("act", lambda e: e.activation(out=wTm[w_i][:, c0:512], in_=ps[sbk][:, c0:512], func=AF.Exp,
                                                     scale=slope, bias=cf[:, bcol:bcol + 1]),
                       reads=[f"ps{sbk}", "cf"], writes=[f"wTm{w_i}"])

            def stB(it):
                qt, kt, nk, idx = it["qt"], it["kt"], it["nk"], it["idx"]
                j = kt - 4 * qt
                w_i = idx % 3
                ab = 4 + (qt % 2)

                def pvmm(e):
                    ins = None
                    for i in range(max(j, 0), 4):
                        ins = e.matmul(ps[ab][:, i * 128:i * 128 + 65], lhsT=wTm[w_i][:, i * 128:(i + 1) * 128],
                                       rhs=V[:, kt, 0:65], start=(kt == 0 and i == 0), stop=(kt == nk - 1),
                                       skip_group_check=True)
                    return ins
                sc.add("pe", pvmm, reads=[f"wTm{w_i}", f"V{kt}", "Vones"], writes=[f"ps{ab}"])
                if kt == nk - 1:
                    accv = ps[ab][:].rearrange("p (i c) -> p i c", i=4)
                    r0 = 4 * (qt % 2)
                    sc.add("dve", lambda e: e.reciprocal(
                        out=rec[:, r0:r0 + 4].rearrange("p (i o) -> p i o", o=1), in_=accv[:, :, 64:65]),
                        reads=[f"ps{ab}"], writes=[f"rec{qt % 2}"])
                    for i in range(4):
                        t = 4 * qt + i
                        sc.add("dve", lambda e, i=i, t=t: e.scalar_tensor_tensor(
                            out=G[:, t, h * 64:(h + 1) * 64], in0=accv[:, i, 0:64], scalar=rec[:, r0 + i:r0 + i + 1],
                            in1=sz[:, t, :], op0=ALU.mult, op1=ALU.mult),
                            reads=[f"ps{ab}", f"rec{qt % 2}", f"sz{t}"], writes=[f"G{t}"])
            run_pipeline(items, [(stA, 0), (stB, 2)], fillers, post)

        def sb_attn(h, par, fillers, vzg):
            qT, kT = qTs[par], kTs[par]
            items = []
            post = {}
            for iq in range(NT):
                nch = (NT - iq + 3) // 4
                for c in range(nch):
                    k0 = 128 * iq + 512 * c
                    N = min(512, S - k0)
                    items.append(dict(iq=iq, c=c, nch=nch, k0=k0, N=N, idx=len(items)))
                if vzg and iq % 4 == 3:
                    post[len(items) - 1] = [vzg[iq // 4]]

            def stA(it):
                iq, c, k0, N, idx = it["iq"], it["c"], it["k0"], it["N"], it["idx"]
                y = idx % 3
                sbk = idx % 2

                def smm(e):
                    ins = e.matmul(ps[sbk][:, 0:N], lhsT=qT[0:64, iq * 128:(iq + 1) * 128], rhs=kT[0:64, k0:k0 + N],
                                   start=True, stop=(c > 0))
                    if c == 0:
                        ins = e.matmul(ps[sbk][:, 0:128], lhsT=ident, rhs=sbpen, start=False, stop=True)
                    return ins
                sc.add("pe", smm, reads=[f"qT{par}", f"kT{par}", "cbf"], writes=[f"ps{sbk}"])
                sc.add("act", lambda e: e.activation(out=abuf[y][:, 0:N], in_=ps[sbk][:, 0:N], func=AF.Sigmoid,
                                                     scale=-1.0),
                       reads=[f"ps{sbk}"], writes=[f"abuf{y}"])

            def stA1(it):
                c, N, idx = it["c"], it["N"], it["idx"]
                y = idx % 3
                yi = idx % 4
                py = (idx - 1) % 4
                init = ones_col if c == 0 else incl[py][:, 512:513]
                rd = ["ones"] if c == 0 else [f"incl{py}"]
                sc.add("dve", lambda e: e.tensor_tensor_scan(
                    out=incl[yi][:, 1:N + 1], data0=abuf[y][:, 0:N], data1=zeros[:, 0:N], initial=init,
                    op0=ALU.mult, op1=ALU.add),
                    reads=[f"abuf{y}", "zeros"] + rd, writes=[f"incl{yi}"])

            def stB1(it):
                c, N, idx = it["c"], it["N"], it["idx"]
                y = idx % 3
                yi = idx % 4
                py = (idx - 1) % 4
                init = ones_col if c == 0 else incl[py][:, 512:513]
                rd = ["ones"] if c == 0 else [f"incl{py}"]
                sc.add("dve", lambda e: e.tensor_tensor(out=wbuf[y][:, 1:N], in0=incl[yi][:, 1:N],
                                                        in1=incl[yi][:, 2:N + 1], op=ALU.subtract),
                       reads=[f"incl{yi}"], writes=[f"wbuf{y}"])
                sc.add("act", lambda e: e.activation(out=wbuf[y][:, 0:1], in_=incl[yi][:, 1:2], func=AF.Identity,
                                                     scale=-1.0, bias=init),
                       reads=[f"incl{yi}"] + rd, writes=[f"wbuf{y}"])

            def stT(it):
                N, idx = it["N"], it["idx"]
                y = idx % 3
                tbk = 2 + (idx % 2)
                nb = N // 128

                def tmm(e):
                    ins = None
                    for b in range(nb):
                        ins = e.matmul(ps[tbk][:, b * 128:(b + 1) * 128], lhsT=wbuf[y][:, b * 128:(b + 1) * 128],
                                       rhs=ident, start=True, stop=True)
                    return ins
                sc.add("pe", tmm, reads=[f"wbuf{y}", "cbf"], writes=[f"ps{tbk}"])

            def stE(it):
                N, idx = it["N"], it["idx"]
                y = idx % 3
                tbk = 2 + (idx % 2)
                sc.add("act", lambda e: e.activation(out=wTs[y][:, 0:N], in_=ps[tbk][:, 0:N], func=AF.Copy),
                       reads=[f"ps{tbk}"], writes=[f"wTs{y}"])

            def stC(it):
                iq, c, nch, N, idx = it["iq"], it["c"], it["nch"], it["N"], it["idx"]
                y = idx % 3
                nb = N // 128
                ab = 4 + (iq % 2)

                def pvmm(e):
                    ins = None
                    for b in range(nb):
                        ins = e.matmul(ps[ab][:, 0:64], lhsT=wTs[y][:, b * 128:(b + 1) * 128],
                                       rhs=V[:, iq + 4 * c + b, 0:64], start=(c == 0 and b == 0),
                                       stop=(c == nch - 1 and b == nb - 1))
                    return ins
                sc.add("pe", pvmm, reads=[f"wTs{y}"] + [f"V{iq + 4 * c + b}" for b in range(nb)], writes=[f"ps{ab}"])
                if c == nch - 1:
                    sc.add("dve", lambda e: e.tensor_tensor(out=G[:, iq, h * 64:(h + 1) * 64], in0=ps[ab][:, 0:64],
                                                            in1=sz[:, iq, :], op=ALU.mult),
                           reads=[f"ps{ab}", f"sz{iq}"], writes=[f"G{iq}"])
            run_pipeline(items, [(stA1, 1), (stE, 4), (stA, 0), (stC, 5), (stB1, 2), (stT, 3)], fillers, post)

        def phase_c(l, hsrc_d):
            sc.add("pool", lambda e: e.dma_start(out=wo, in_=wout_d[l], max_dma_last_dim=8192),
                   writes=["wo"], dma_key="wo")
            load_gb(l + 1)
            rev = (l == 1)
            mat = Jm if rev else ident

            def c1(m):
                nat = (NT - 1 - m) if rev else m
                s_ = m % 4
                g_ = m % 2
                sc.add("sp", lambda e: e.dma_start(out=xt[s_], in_=hsrc_d[nat * 128:(nat + 1) * 128, :]),
                       writes=[f"xt{s_}"], dma_key=f"xt{s_}")

                def tr(e, half):
                    ins = None
                    for c in range(4):
                        kc = half * 4 + c
                        ins = e.matmul(ps[half][:, c * 128:(c + 1) * 128], lhsT=G[:, m, kc * 128:(kc + 1) * 128],
                                       rhs=mat, start=True, stop=True)
                    return ins
                for half in range(2):
                    sc.add("pe", lambda e, half=half: tr(e, half), reads=[f"G{m}", "cbf"], writes=[f"ps{half}"])
                sc.add("act", lambda e: e.activation(out=GT[g_][:, 0:4, :],
                                                     in_=ps[0][:].rearrange("p (k c) -> p k c", k=4), func=AF.Copy),
                       reads=["ps0"], writes=[f"GT{g_}"])
                sc.add("dve", lambda e: e.tensor_copy(out=GT[g_][:, 4:8, :],
                                                      in_=ps[1][:].rearrange("p (k c) -> p k c", k=4)),
                       reads=["ps1"], writes=[f"GT{g_}"])

            def c2(m):
                s_ = m % 4
                g_ = m % 2
                for half in range(2):
                    def omm(e, half=half):
                        ins = None
                        for kc in range(8):
                            ins = e.matmul(ps[2 + half][:, :], lhsT=GT[g_][:, kc, :],
                                           rhs=wo[:, kc, half * 512:(half + 1) * 512], start=(kc == 0), stop=(kc == 7))
                        return ins
                    sc.add("pe", omm, reads=[f"GT{g_}", "wo"], writes=[f"ps{2 + half}"])
                    sc.add("dve", lambda e, half=half: e.tensor_tensor(
                        out=xt[s_][:, half * 512:(half + 1) * 512], in0=ps[2 + half][:, :],
                        in1=xt[s_][:, half * 512:(half + 1) * 512], op=ALU.add),
                        reads=[f"ps{2 + half}", f"xt{s_}"], writes=[f"xt{s_}"])

            def c3(m):
                nat = (NT - 1 - m) if rev else m
                s_ = m % 4
                if l == 0:
                    sc.add("sp", lambda e: e.dma_start(out=h1_d[nat * 128:(nat + 1) * 128, :], in_=xt[s_]),
                           reads=[f"xt{s_}"], writes=[f"h1_{nat}"], dma_key=f"st{s_}")
                    if 1 in layers:
                        norm_part(s_)
                else:
                    norm_part(s_, final=True)

            def c4(m):
                nat = (NT - 1 - m) if rev else m
                s_ = m % 4
                if l == 0:
                    if 1 in layers:
                        tr_part(s_, NT - 1 - nat, True, 4)
                else:
                    sc.add("sp", lambda e: e.dma_start(out=out_d[nat * 128:(nat + 1) * 128, :], in_=xt[s_]),
                           reads=[f"xt{s_}"], writes=[f"out_{nat}"], dma_key=f"st{s_}")
            run_pipeline(list(range(NT)), [(c1, 0), (c2, 1), (c3, 2), (c4, 3)])

        for l in layers:
            moba = (l == 0)
            if l == 1 and 0 not in layers:
                raise NotImplementedError
            load_wh(l, heads[0], 0)
            if stop != "A":
                for grp in qk_groups(l, heads[0], 0, moba, 0):
                    grp()
            if stop != "A":
                for grp in vz_groups(l, heads[0], 0):
                    grp()
            for hi, h in enumerate(heads):
                if stop == "A":
                    continue
                par = hi % 2
                fill = []
                vzg = None
                if hi + 1 < len(heads):
                    load_wh(l, heads[hi + 1], (hi + 1) % 2)
                    fill = qk_groups(l, heads[hi + 1], (hi + 1) % 2, moba, 1 - par)
                    vzg = vz_groups(l, heads[hi + 1], (hi + 1) % 2)
                if moba:
                    moba_attn(h, par, fill, vzg)
                else:
                    sb_attn(h, par, fill, vzg)
            sc.barrier()
            if debug and l == layers[-1]:
                sc.add("sp", lambda e: e.dma_start(out=gd_d[:, :, :], in_=G[:, :, :]), reads=[], writes=["gd"], dma_key="stg")
                sc.barrier()
            if stop in (None, "C"):
                phase_c(l, x_d if l == 0 else h1_d)
            sc.barrier()
        if 1 not in layers:
            pass

        fin = Op("sp", None)
        fin.deps = [o for k, o in sc.all_dma_last.items() if k.startswith("st")]
        sc.ops["sp"].append(fin)

        sc.finalize()
        esems = {e: [st.enter_context(nc.semaphore(f"s_{e}{i}")) for i in range(sc.nsem[e])] for e in ENGS}
        dsems = {k: st.enter_context(nc.semaphore(f"d_{k}")) for k in sc.dma_cnt}
        block = st.enter_context(nc.Block())

        @block.tensor
        def _(e):
            sc.emit("pe", e, esems, dsems)

        @block.scalar
        def _(e):
            sc.emit("act", e, esems, dsems)

        @block.vector
        def _(e):
            sc.emit("dve", e, esems, dsems)

        @block.gpsimd
        def _(e):
            sc.emit("pool", e, esems, dsems)

        @block.sync
        def _(e):
            sc.emit("sp", e, esems, dsems)
    return nc


def _consts():
    cbf = np.zeros((128, 512), np.float32)
    p = np.arange(128)
    cbf[p, p] = 1.0
    cbf[p, 128 + (127 - p)] = 1.0
    cbf[:, 256:384] = np.where(p[:, None] > p[None, :], -BIGPEN, 0.0)
    cbf[:, 384:512] = np.where(p[None, :] <= p[:, None], -SBPEN, 0.0)
    cf = np.zeros((128, 512), np.float32)
    for h in range(H):
        for idx in range(32):
            cf[:, h * 32 + idx] = np.float32(SLOPES[h]) * (128.0 * (idx - 28) + p)
    t = np.arange(S)
    augk = np.zeros((32, S), np.float32)
    augk[t // 256, t] = 1.0
    augk[16:18, :] = 1.0
    augq = np.zeros((16, S), np.float32)
    augq[0] = -128.0 * ((t % 512) // 128)
    augq[1] = -(t % 128)
    return cbf, cf, augk, augq


_NC_CACHE = {}


def kernel(x, norm_g, w_in, w_out, final_g):
    x = np.asarray(x, np.float32)
    w_in = np.asarray(w_in, np.float32)
    w_out = np.asarray(w_out, np.float32)
    norm_g = np.asarray(norm_g, np.float32)
    final_g = np.asarray(final_g, np.float32)
    B = x.shape[0]
    wi = w_in.reshape(2, 8, 128, 4, H, DH)
    win = np.ascontiguousarray(wi.transpose(0, 4, 2, 1, 3, 5)).reshape(2, H, 128, 8, 256)
    wout = np.ascontiguousarray(w_out.reshape(2, 8, 128, D).transpose(0, 2, 1, 3))
    gb = np.ascontiguousarray(np.broadcast_to(
        np.concatenate([norm_g, final_g[None, :]], axis=0)[:, None, :], (3, 128, D)))
    cbf, cf, augk, augq = _consts()
    if "nc" not in _NC_CACHE:
        _NC_CACHE["nc"] = build_program()
    nc = _NC_CACHE["nc"]
    in_maps = [{"x": np.ascontiguousarray(x[b]), "win": win, "wout": wout, "gb": gb, "cbf": cbf, "cf": cf,
                "augk": augk, "augq": augq} for b in range(B)]
    res = run_bass_kernel_spmd(nc, in_maps, core_ids=list(range(B)))
    return np.stack([np.asarray(r["out"]) for r in res.results], axis=0).astype(np.float32)
```

```python
import contextlib
import numpy as np
import concourse.bass as bass
import concourse.mybir as mybir
from concourse.bass_utils import run_bass_kernel_spmd

F32 = mybir.dt.float32
BF16 = mybir.dt.bfloat16
AF = mybir.ActivationFunctionType
ALU = mybir.AluOpType
AX = mybir.AxisListType

S = 4096
D = 1024
H = 16
DH = 64
NT = S // 128
EPS = 1e-6
BIGPEN = 65536.0
SBPEN = 32768.0
SLOPES = [2.0 ** (-(h + 1) / 2.0) for h in range(H)]

ENGS = ("pe", "act", "dve", "pool", "sp")
ROT = 8000


class Op:
    __slots__ = ("eng", "fn", "deps", "signal", "seq", "semi", "is_dma", "key", "cnt", "idx")

    def __init__(self, eng, fn, is_dma=False, key=None):
        self.eng = eng
        self.fn = fn
        self.deps = []
        self.signal = False
        self.seq = 0
        self.semi = 0
        self.is_dma = is_dma
        self.key = key
        self.cnt = 0


class Sched:
    def __init__(self):
        self.ops = {e: [] for e in ENGS}
        self.lastw = {}
        self.readers = {}
        self.dma_cnt = {}
        self.all_dma_last = {}

    def add(self, eng, fn, reads=(), writes=(), dma_key=None):
        op = Op(eng, fn, is_dma=dma_key is not None, key=dma_key)
        deps = {}

        def dep(o, kind):
            if o is None or o is op:
                return
            if not o.is_dma and not op.is_dma and o.eng == eng:
                if eng == "pe":
                    return
            deps[id(o)] = o

        rset = []
        wset = list(writes)
        for r in reads:
            if r.startswith("ps"):
                wset.append(r)
            else:
                rset.append(r)
        for r in rset:
            dep(self.lastw.get(r), "raw")
        for w in wset:
            dep(self.lastw.get(w), "raw" if w.startswith("ps") else "waw")
            for o in self.readers.get(w, {}).values():
                dep(o, "war")
        op.deps = list(deps.values())
        for o in op.deps:
            o.signal = True
        for r in rset:
            d = self.readers.setdefault(r, {})
            d[("dma", id(op)) if op.is_dma else eng] = op
        for w in wset:
            self.lastw[w] = op
            self.readers[w] = {}
        if op.is_dma:
            self.dma_cnt[dma_key] = self.dma_cnt.get(dma_key, 0) + 1
            op.cnt = self.dma_cnt[dma_key]
            self.all_dma_last[dma_key] = op
        self.ops[eng].append(op)
        return op

    def barrier(self):
        lasts = []
        for e in ENGS:
            for o in reversed(self.ops[e]):
                if not o.is_dma and o.fn is not None:
                    lasts.append(o)
                    break
        lasts += list(self.all_dma_last.values())
        for e in ENGS:
            op = Op(e, None)
            op.deps = [o for o in lasts if o.is_dma or o.eng != e]
            for o in op.deps:
                o.signal = True
            self.ops[e].append(op)

    def finalize(self):
        self.nsem = {}
        for e in ENGS:
            c = 0
            for o in self.ops[e]:
                if o.signal and not o.is_dma:
                    o.semi = c // ROT
                    o.seq = c % ROT + 1
                    c += 1
            self.nsem[e] = max(1, (c + ROT - 1) // ROT)

    def emit(self, eng, e, esems, dsems):
        waited = {}
        for o in self.ops[eng]:
            for d in o.deps:
                if d.is_dma:
                    k = ("d", d.key)
                    v = 16 * d.cnt
                    sem = dsems[d.key]
                else:
                    k = (d.eng, d.semi)
                    v = d.seq
                    sem = esems[d.eng][d.semi]
                if waited.get(k, 0) >= v:
                    continue
                waited[k] = v
                e.wait_ge(sem, v)
            if o.fn is None:
                continue
            ins = o.fn(e)
            if o.is_dma:
                ins.then_inc(dsems[o.key], 16)
            elif o.signal:
                ins.then_inc(esems[eng][o.semi], 1)


def build_program(layers=(0, 1), debug=False, heads=None, stop=None):
    heads = list(range(H)) if heads is None else list(heads)
    nc = bass.Bass("TRN2", target_bir_lowering=False)
    x_d = nc.dram_tensor("x", [S, D], F32, kind="ExternalInput").ap()
    win_d = nc.dram_tensor("win", [2, H, 128, 8, 256], F32, kind="ExternalInput").ap()
    wout_d = nc.dram_tensor("wout", [2, 128, 8, D], F32, kind="ExternalInput").ap()
    gb_d = nc.dram_tensor("gb", [3, 128, D], F32, kind="ExternalInput").ap()
    cbf_d = nc.dram_tensor("cbf", [128, 512], F32, kind="ExternalInput").ap()
    cf_d = nc.dram_tensor("cf", [128, 512], F32, kind="ExternalInput").ap()
    augk_d = nc.dram_tensor("augk", [32, S], F32, kind="ExternalInput").ap()
    augq_d = nc.dram_tensor("augq", [16, S], F32, kind="ExternalInput").ap()
    out_d = nc.dram_tensor("out", [S, D], F32, kind="ExternalOutput").ap()
    if debug:
        h1_d = nc.dram_tensor("h1", [S, D], F32, kind="ExternalOutput").ap()
        gd_d = nc.dram_tensor("gd", [128, NT, D], BF16, kind="ExternalOutput").ap()
    else:
        h1_d = nc.dram_tensor("h1", [S, D], F32, kind="Internal").ap()

    sc = Sched()
    st = contextlib.ExitStack()
    with st:
        def sb(name, shape, dt):
            return st.enter_context(nc.sbuf_tensor("sb_" + name, shape, dt))

        hnT = sb("hnT", [128, 8, S], BF16)
        G = sb("G", [128, NT, D], BF16)
        cbf = sb("cbf", [128, 512], BF16)
        cf = sb("cf", [128, 512], F32)
        zeros = sb("zeros", [128, 512], F32)
        penpad = sb("penpad", [128, 4, 128], BF16)
        small = sb("small", [128, 64], F32)
        kms = sb("kms", [128, 16], F32)
        kmb = sb("kmb", [128, 16], BF16)
        gsb = sb("gsb", [128, 64], F32)
        m8 = sb("m8", [128, 32], F32)
        rec = sb("rec", [128, 8], F32)
        POOLW = 18600
        pool = sb("pool", [128, POOLW], F32)
        off = [0]

        def carve(words, dt=F32):
            a = off[0]
            off[0] += words
            assert off[0] <= POOLW, off[0]
            v = pool[:, a:a + words]
            if dt is BF16:
                v = v.bitcast(BF16)
            return v

        qkb = [carve(4096, BF16) for _ in range(2)]
        qTs = [b[:, 0:S] for b in qkb]
        kTs = [b[:, S:2 * S] for b in qkb]
        wh = [carve(1024, BF16).rearrange("p (k c) -> p k c", k=8) for _ in range(2)]
        V = carve(NT * 36, BF16).rearrange("p (t c) -> p t c", t=NT)
        sz = carve(NT * 64, F32).rearrange("p (t c) -> p t c", t=NT)
        wk0 = off[0]
        wTm = [carve(256, BF16) for _ in range(4)]
        off[0] = wk0
        abuf = [carve(512, F32) for _ in range(3)]
        incl = [carve(516, F32) for _ in range(4)]
        wbuf = [carve(256, BF16) for _ in range(3)]
        wTs = [carve(256, BF16) for _ in range(3)]
        endB = off[0]
        off[0] = 0
        wo = carve(4096, BF16).rearrange("p (k c) -> p k c", k=8)
        xt = [carve(1024, F32) for _ in range(4)]
        hn = [carve(512, BF16) for _ in range(4)]
        gbc = carve(1024, F32)
        GT = [carve(512, BF16).rearrange("p (k c) -> p k c", k=8) for _ in range(2)]
        junk = carve(512, BF16)
        assert off[0] <= POOLW

        ps = [st.enter_context(nc.psum_tensor(f"ps{i}", [128, 512], F32)) for i in range(8)]

        ident = cbf[:, 0:128]
        Jm = cbf[:, 128:256]
        tripen = cbf[:, 256:384]
        sbpen = cbf[:, 384:512]
        ss = small[:, 16:20]
        lnv = small[:, 20:24]
        rstd = small[:, 24:28]

        sc.add("pool", lambda e: e.dma_start(out=cbf[:], in_=cbf_d[:, :]), writes=["cbf"], dma_key="cst")
        sc.add("sp", lambda e: e.dma_start(out=cf[:], in_=cf_d[:, :]), writes=["cf"], dma_key="cst2")
        sc.add("pool", lambda e: e.memset(zeros[:], 0.0), writes=["zeros"])
        if debug:
            for t in range(NT):
                sc.add("pool", lambda e, t=t: e.memset(G[:, t, :], 0.0), writes=[f"G{t}"])
        sc.add("pool", lambda e: e.memset(penpad[:], 0.0), writes=["penpad"])

        def run_pipeline(items, stages, fillers=(), post=None):
            n = len(items)
            maxoff = max(o for _, o in stages)
            nsteps = n + maxoff
            fillers = list(fillers)
            nf = len(fillers)
            lo, hi = 8, max(9, nsteps - 8)
            fpos = {}
            for fi in range(nf):
                fpos.setdefault(lo + (fi * (hi - lo)) // max(nf, 1), []).append(fillers[fi])
            for i in range(nsteps):
                for fn, offk in stages:
                    j = i - offk
                    if 0 <= j < n:
                        fn(items[j])
                for f in fpos.get(i, ()):
                    f()
                if post:
                    for f in post.get(i - maxoff, ()):
                        f()

        def load_gb(i):
            sc.add("sp", lambda e: e.dma_start(out=gbc, in_=gb_d[i]), writes=["gbc"], dma_key="gbc")

        def hres(t0, n=1):
            return [f"hnT{t}" for t in range(t0, t0 + n)]

        def norm_part(s, final=False):
            sc.add("act", lambda e: e.activation(out=junk, in_=xt[s], func=AF.Square, accum_out=ss[:, s:s + 1]),
                   reads=[f"xt{s}"], writes=["junk", f"ss{s}"])
            sc.add("act", lambda e: e.activation(out=lnv[:, s:s + 1], in_=ss[:, s:s + 1], func=AF.Ln,
                                                 scale=1.0 / D, bias=eps_ap),
                   reads=[f"ss{s}", "eps"], writes=[f"lnv{s}"])
            sc.add("act", lambda e: e.activation(out=rstd[:, s:s + 1], in_=lnv[:, s:s + 1], func=AF.Exp, scale=-0.5),
                   reads=[f"lnv{s}"], writes=[f"rstd{s}"])
            if final:
                sc.add("dve", lambda e: e.scalar_tensor_tensor(out=xt[s], in0=xt[s], scalar=rstd[:, s:s + 1], in1=gbc,
                                                               op0=ALU.mult, op1=ALU.mult),
                       reads=[f"xt{s}", f"rstd{s}", "gbc"], writes=[f"xt{s}"])
            else:
                sc.add("dve", lambda e: e.scalar_tensor_tensor(out=hn[s], in0=xt[s], scalar=rstd[:, s:s + 1], in1=gbc,
                                                               op0=ALU.mult, op1=ALU.mult),
                       reads=[f"xt{s}", f"rstd{s}", "gbc"], writes=[f"hn{s}"])

        def tr_part(s, dst_tile, rev, psb):
            mat = Jm if rev else ident

            def tr(e, half):
                ins = None
                for c in range(4):
                    kc = half * 4 + c
                    ins = e.matmul(ps[psb + half][:, c * 128:(c + 1) * 128], lhsT=hn[s][:, kc * 128:(kc + 1) * 128],
                                   rhs=mat, start=True, stop=True)
                return ins
            for half in range(2):
                sc.add("pe", lambda e, half=half: tr(e, half), reads=[f"hn{s}", "cbf"], writes=[f"ps{psb + half}"])
            cols = slice(dst_tile * 128, (dst_tile + 1) * 128)
            sc.add("act", lambda e: e.activation(out=hnT[:, 0:4, cols], in_=ps[psb][:].rearrange("p (k c) -> p k c", k=4),
                                                 func=AF.Copy),
                   reads=[f"ps{psb}"], writes=hres(dst_tile))
            sc.add("dve", lambda e: e.tensor_copy(out=hnT[:, 4:8, cols],
                                                  in_=ps[psb + 1][:].rearrange("p (k c) -> p k c", k=4)),
                   reads=[f"ps{psb + 1}"], writes=hres(dst_tile))

        eps_ap = small[:, 8:9]
        ones_col = small[:, 9:10]
        sc.add("dve", lambda e: e.memset(eps_ap, EPS), writes=["eps"])
        sc.add("dve", lambda e: e.memset(ones_col, 1.0), writes=["ones"])

        if 0 in layers:
            load_gb(0)
            def a1(t):
                s_ = t % 4
                sc.add("sp", lambda e: e.dma_start(out=xt[s_], in_=x_d[t * 128:(t + 1) * 128, :]),
                       writes=[f"xt{s_}"], dma_key=f"xt{s_}")
                norm_part(s_)

            def a2(t):
                tr_part(t % 4, t, False, (t % 2) * 2)
            run_pipeline(list(range(NT)), [(a1, 0), (a2, 1)])
            sc.barrier()
            for par in range(2):
                sc.add("pool", lambda e, par=par: e.dma_start(out=kTs[par][64:96, :], in_=augk_d[:, :],
                                                              max_dma_last_dim=4096),
                       writes=[f"kTaug{par}"], dma_key=f"cstk{par}")
                sc.add("pool", lambda e, par=par: e.dma_start(out=qTs[par][80:96, :], in_=augq_d[:, :],
                                                              max_dma_last_dim=4096),
                       writes=[f"qTaug{par}"], dma_key=f"cstq{par}")
            sc.add("dve", lambda e: e.memset(V[:, :, 64:65], 1.0), writes=["Vones"])

        def load_wh(l, h, sl):
            sc.add("pool", lambda e: e.dma_start(out=wh[sl], in_=win_d[l, h], max_dma_last_dim=4096),
                   writes=[f"wh{sl}"], dma_key=f"wh{sl}")

        def qk_groups(l, h, sl, moba, par):
            w = wh[sl]
            qT, kT = qTs[par], kTs[par]
            qscale = (0.125 / SLOPES[h]) if moba else 0.125
            groups = []
            for which in range(2):
                for tt in range(8):
                    def grp(which=which, tt=tt):
                        bank = 6 + (tt % 2)

                        def mm(e):
                            ins = None
                            for kc in range(8):
                                ins = e.matmul(ps[bank][0:64, :], lhsT=w[:, kc, which * 64:(which + 1) * 64],
                                               rhs=hnT[:, kc, tt * 512:(tt + 1) * 512], start=(kc == 0),
                                               stop=(kc == 7))
                            return ins
                        sc.add("pe", mm, reads=[f"wh{sl}"] + hres(tt * 4, 4), writes=[f"ps{bank}"])
                        cols = slice(tt * 512, (tt + 1) * 512)
                        if which == 0:
                            if moba:
                                sc.add("dve", lambda e: e.tensor_scalar(out=qT[0:64, cols], in0=ps[bank][0:64, :],
                                                                        scalar1=qscale, scalar2=None, op0=ALU.mult),
                                       reads=[f"ps{bank}"], writes=[f"qT{par}"])
                            else:
                                sc.add("act", lambda e: e.activation(out=qT[0:64, cols], in_=ps[bank][0:64, :],
                                                                     func=AF.Copy, scale=qscale),
                                       reads=[f"ps{bank}"], writes=[f"qT{par}"])
                        else:
                            if moba:
                                sc.add("dve", lambda e: e.tensor_reduce(
                                    out=kms[0:64, 2 * tt:2 * tt + 2],
                                    in_=ps[bank][0:64, :].rearrange("p (a b) -> p a b", a=2), axis=AX.X, op=ALU.add),
                                    reads=[f"ps{bank}"], writes=["kms"])
                                sc.add("dve", lambda e: e.tensor_copy(out=kT[0:64, cols], in_=ps[bank][0:64, :]),
                                       reads=[f"ps{bank}"], writes=[f"kT{par}"])
                            else:
                                sc.add("act", lambda e: e.activation(out=kT[0:64, cols], in_=ps[bank][0:64, :],
                                                                     func=AF.Copy),
                                       reads=[f"ps{bank}"], writes=[f"kT{par}"])
                    groups.append(grp)
            if moba:
                def g0():
                    sc.add("dve", lambda e: e.tensor_scalar(out=kmb[0:64, :], in0=kms[0:64, :], scalar1=1.0 / 256.0,
                                                            scalar2=None, op0=ALU.mult),
                           reads=["kms"], writes=["kmb"])
                groups.append(g0)
                for g in range(8):
                    def ggrp(g=g):
                        def gmm(e):
                            ins = None
                            for i in range(4):
                                t = 4 * g + i
                                ins = e.matmul(ps[2][:, i * 16:(i + 1) * 16], lhsT=qT[0:64, t * 128:(t + 1) * 128],
                                               rhs=kmb[0:64, :], start=True, stop=True)
                            return ins
                        sc.add("pe", gmm, reads=[f"qT{par}", "kmb"], writes=["ps2"])
                        for i in range(4):
                            t = 4 * g + i
                            cur = t // 2
                            gs = gsb[:, i * 16:(i + 1) * 16]
                            sc.add("dve", lambda e, gs=gs: e.memset(gs, -3.0e38), writes=[f"gsb{i}"])
                            if cur > 0:
                                sc.add("dve", lambda e, i=i, cur=cur, gs=gs: e.tensor_copy(
                                    out=gs[:, 0:cur], in_=ps[2][:, i * 16:i * 16 + cur]),
                                    reads=["ps2"], writes=[f"gsb{i}"])
                            sc.add("dve", lambda e, i=i, gs=gs: e.max(out=m8[:, i * 8:(i + 1) * 8], in_=gs),
                                   reads=[f"gsb{i}"], writes=[f"m8{i}"])
                            sc.add("dve", lambda e, i=i, gs=gs: e.tensor_scalar(
                                out=penpad[:, i, 64:80], in0=gs, scalar1=m8[:, i * 8 + 2:i * 8 + 3],
                                scalar2=-BIGPEN, op0=ALU.is_lt, op1=ALU.mult),
                                reads=[f"gsb{i}", f"m8{i}"], writes=[f"penpad{i}"])
                            sc.add("dve", lambda e, i=i, cur=cur: e.memset(penpad[:, i, 64 + cur:65 + cur], 0.0),
                                   reads=[f"penpad{i}"], writes=[f"penpad{i}"])

                        def pmm(e):
                            ins = None
                            for i in range(4):
                                ins = e.matmul(ps[3][:, i * 128:(i + 1) * 128], lhsT=penpad[:, i, :], rhs=ident,
                                               start=True, stop=True)
                            return ins
                        sc.add("pe", pmm, reads=[f"penpad{i}" for i in range(4)] + ["penpad", "cbf"], writes=["ps3"])
                        sc.add("act", lambda e: e.activation(out=qT[64:80, g * 512:(g + 1) * 512], in_=ps[3][64:80, :],
                                                             func=AF.Copy),
                               reads=["ps3"], writes=[f"qT{par}"])
                    groups.append(ggrp)
            return groups

        def vz_groups(l, h, sl):
            w = wh[sl]
            groups = []
            for g in range(8):
                def grp(g=g):
                    bank = 6 + (g % 2)

                    def mmvz(e):
                        ins = None
                        for i in range(4):
                            t = 4 * g + i
                            for kc in range(8):
                                ins = e.matmul(ps[bank][:, i * 128:(i + 1) * 128],
                                               lhsT=hnT[:, kc, t * 128:(t + 1) * 128],
                                               rhs=w[:, kc, 128:256], start=(kc == 0), stop=(kc == 7))
                        return ins
                    sc.add("pe", mmvz, reads=[f"wh{sl}"] + hres(4 * g, 4), writes=[f"ps{bank}"])
                    pv = ps[bank][:].rearrange("p (i c) -> p i c", i=4)
                    sc.add("dve", lambda e: e.tensor_copy(out=V[:, 4 * g:4 * g + 4, 0:64], in_=pv[:, :, 0:64]),
                           reads=[f"ps{bank}"], writes=[f"V{4 * g + i}" for i in range(4)])
                    sc.add("act", lambda e: e.activation(out=sz[:, 4 * g:4 * g + 4, :], in_=pv[:, :, 64:128],
                                                         func=AF.Silu),
                           reads=[f"ps{bank}"], writes=[f"sz{4 * g + i}" for i in range(4)])
                groups.append(grp)
            return groups

        def moba_attn(h, par, fillers, vzg):
            slope = SLOPES[h]
            qT, kT = qTs[par], kTs[par]
            items = []
            post = {}
            for qt in range(7, -1, -1):
                nk = 4 * qt + 4
                for kt in range(nk):
                    items.append(dict(qt=qt, kt=kt, nk=nk, idx=len(items)))
                if vzg:
                    post[len(items) - 1] = [vzg[qt]]

            def stA(it):
                qt, kt, idx = it["qt"], it["kt"], it["idx"]
                j = kt - 4 * qt
                c0 = 128 * max(j, 0)
                sbk = idx % 2
                w_i = idx % 4

                def smm(e):
                    ins = e.matmul(ps[sbk][:, c0:512], lhsT=kT[0:82, kt * 128:(kt + 1) * 128],
                                   rhs=qT[0:82, qt * 512 + c0:(qt + 1) * 512], start=True, stop=(j < 0))
                    if j >= 0:
                        ins = e.matmul(ps[sbk][:, c0:c0 + 128], lhsT=ident, rhs=tripen, start=False, stop=True)
                    return ins
                sc.add("pe", smm, reads=[f"qT{par}", f"kT{par}", f"qTaug{par}", f"kTaug{par}", "cbf"],
                       writes=[f"ps{sbk}"])
                bcol = h * 32 + (j + 28)
                sc.add("act", lambda e: e.activation(out=wTm[w_i][:, c0:512], in_=ps[sbk][:, c0:512], func=AF.Exp,
                                                     scale=slope, bias=cf[:, bcol:bcol + 1]),
                       reads=[f"ps{sbk}", "cf"], writes=[f"wTm{w_i}"])

            def stB(it):
                qt, kt, nk, idx = it["qt"], it["kt"], it["nk"], it["idx"]
                j = kt - 4 * qt
                w_i = idx % 4
                ab = 4 + (qt % 2)

                def pvmm(e):
                    ins = None
                    for i in range(max(j, 0), 4):
                        ins = e.matmul(ps[ab][:, i * 128:i * 128 + 65], lhsT=wTm[w_i][:, i * 128:(i + 1) * 128],
                                       rhs=V[:, kt, 0:65], start=(kt == 0 and i == 0), stop=(kt == nk - 1),
                                       skip_group_check=True)
                    return ins
                sc.add("pe", pvmm, reads=[f"wTm{w_i}", f"V{kt}", "Vones"], writes=[f"ps{ab}"])
                if kt == nk - 1:
                    accv = ps[ab][:].rearrange("p (i c) -> p i c", i=4)
                    r0 = 4 * (qt % 2)
                    sc.add("dve", lambda e: e.reciprocal(
                        out=rec[:, r0:r0 + 4].rearrange("p (i o) -> p i o", o=1), in_=accv[:, :, 64:65]),
                        reads=[f"ps{ab}"], writes=[f"rec{qt % 2}"])
                    for i in range(4):
                        t = 4 * qt + i
                        sc.add("dve", lambda e, i=i, t=t: e.scalar_tensor_tensor(
                            out=G[:, t, h * 64:(h + 1) * 64], in0=accv[:, i, 0:64], scalar=rec[:, r0 + i:r0 + i + 1],
                            in1=sz[:, t, :], op0=ALU.mult, op1=ALU.mult),
                            reads=[f"ps{ab}", f"rec{qt % 2}", f"sz{t}"], writes=[f"G{t}"])
            run_pipeline(items, [(stA, 0), (stB, 2)], fillers, post)

        def sb_attn(h, par, fillers, vzg):
            qT, kT = qTs[par], kTs[par]
            items = []
            post = {}
            for iq in range(NT):
                nch = (NT - iq + 3) // 4
                for c in range(nch):
                    k0 = 128 * iq + 512 * c
                    N = min(512, S - k0)
                    items.append(dict(iq=iq, c=c, nch=nch, k0=k0, N=N, idx=len(items)))
                if vzg and iq % 4 == 3:
                    post[len(items) - 1] = [vzg[iq // 4]]

            def stA(it):
                iq, c, k0, N, idx = it["iq"], it["c"], it["k0"], it["N"], it["idx"]
                y = idx % 3
                sbk = idx % 2

                def smm(e):
                    ins = e.matmul(ps[sbk][:, 0:N], lhsT=qT[0:64, iq * 128:(iq + 1) * 128], rhs=kT[0:64, k0:k0 + N],
                                   start=True, stop=(c > 0))
                    if c == 0:
                        ins = e.matmul(ps[sbk][:, 0:128], lhsT=ident, rhs=sbpen, start=False, stop=True)
                    return ins
                sc.add("pe", smm, reads=[f"qT{par}", f"kT{par}", "cbf"], writes=[f"ps{sbk}"])
                sc.add("act", lambda e: e.activation(out=abuf[y][:, 0:N], in_=ps[sbk][:, 0:N], func=AF.Sigmoid,
                                                     scale=-1.0),
                       reads=[f"ps{sbk}"], writes=[f"abuf{y}"])

            def stA1(it):
                c, N, idx = it["c"], it["N"], it["idx"]
                y = idx % 3
                yi = idx % 4
                py = (idx - 1) % 4
                init = ones_col if c == 0 else incl[py][:, 512:513]
                rd = ["ones"] if c == 0 else [f"incl{py}"]
                sc.add("dve", lambda e: e.tensor_tensor_scan(
                    out=incl[yi][:, 1:N + 1], data0=abuf[y][:, 0:N], data1=zeros[:, 0:N], initial=init,
                    op0=ALU.mult, op1=ALU.add),
                    reads=[f"abuf{y}", "zeros"] + rd, writes=[f"incl{yi}"])

            def stB1(it):
                c, N, idx = it["c"], it["N"], it["idx"]
                y = idx % 3
                yi = idx % 4
                py = (idx - 1) % 4
                init = ones_col if c == 0 else incl[py][:, 512:513]
                rd = ["ones"] if c == 0 else [f"incl{py}"]
                sc.add("dve", lambda e: e.tensor_tensor(out=wbuf[y][:, 1:N], in0=incl[yi][:, 1:N],
                                                        in1=incl[yi][:, 2:N + 1], op=ALU.subtract),
                       reads=[f"incl{yi}"], writes=[f"wbuf{y}"])
                sc.add("act", lambda e: e.activation(out=wbuf[y][:, 0:1], in_=incl[yi][:, 1:2], func=AF.Identity,
                                                     scale=-1.0, bias=init),
                       reads=[f"incl{yi}"] + rd, writes=[f"wbufc{y}"])

            def stT(it):
                N, idx = it["N"], it["idx"]
                y = idx % 3
                tbk = 2 + (idx % 2)
                nb = N // 128

                def tmm(e):
                    ins = None
                    for b in range(nb):
                        ins = e.matmul(ps[tbk][:, b * 128:(b + 1) * 128], lhsT=wbuf[y][:, b * 128:(b + 1) * 128],
                                       rhs=ident, start=True, stop=True)
                    return ins
                sc.add("pe", tmm, reads=[f"wbuf{y}", f"wbufc{y}", "cbf"], writes=[f"ps{tbk}"])

            def stE(it):
                N, idx = it["N"], it["idx"]
                y = idx % 3
                tbk = 2 + (idx % 2)
                sc.add("act", lambda e: e.activation(out=wTs[y][:, 0:N], in_=ps[tbk][:, 0:N], func=AF.Copy),
                       reads=[f"ps{tbk}"], writes=[f"wTs{y}"])

            def stC(it):
                iq, c, nch, N, idx = it["iq"], it["c"], it["nch"], it["N"], it["idx"]
                y = idx % 3
                nb = N // 128
                ab = 4 + (iq % 2)

                def pvmm(e):
                    ins = None
                    for b in range(nb):
                        ins = e.matmul(ps[ab][:, 0:64], lhsT=wTs[y][:, b * 128:(b + 1) * 128],
                                       rhs=V[:, iq + 4 * c + b, 0:64], start=(c == 0 and b == 0),
                                       stop=(c == nch - 1 and b == nb - 1))
                    return ins
                sc.add("pe", pvmm, reads=[f"wTs{y}"] + [f"V{iq + 4 * c + b}" for b in range(nb)], writes=[f"ps{ab}"])
                if c == nch - 1:
                    sc.add("dve", lambda e: e.tensor_tensor(out=G[:, iq, h * 64:(h + 1) * 64], in0=ps[ab][:, 0:64],
                                                            in1=sz[:, iq, :], op=ALU.mult),
                           reads=[f"ps{ab}", f"sz{iq}"], writes=[f"G{iq}"])
            run_pipeline(items, [(stA1, 1), (stE, 4), (stA, 0), (stC, 5), (stB1, 2), (stT, 3)], fillers, post)

        def phase_c(l, hsrc_d):
            sc.add("pool", lambda e: e.dma_start(out=wo, in_=wout_d[l], max_dma_last_dim=8192),
                   writes=["wo"], dma_key="wo")
            load_gb(l + 1)
            rev = (l == 1)
            mat = Jm if rev else ident

            def c1(m):
                nat = (NT - 1 - m) if rev else m
                s_ = m % 4
                g_ = m % 2
                sc.add("sp", lambda e: e.dma_start(out=xt[s_], in_=hsrc_d[nat * 128:(nat + 1) * 128, :]),
                       writes=[f"xt{s_}"], dma_key=f"xt{s_}")

                def tr(e, half):
                    ins = None
                    for c in range(4):
                        kc = half * 4 + c
                        ins = e.matmul(ps[half][:, c * 128:(c + 1) * 128], lhsT=G[:, m, kc * 128:(kc + 1) * 128],
                                       rhs=mat, start=True, stop=True)
                    return ins
                for half in range(2):
                    sc.add("pe", lambda e, half=half: tr(e, half), reads=[f"G{m}", "cbf"], writes=[f"ps{half}"])
                sc.add("act", lambda e: e.activation(out=GT[g_][:, 0:4, :],
                                                     in_=ps[0][:].rearrange("p (k c) -> p k c", k=4), func=AF.Copy),
                       reads=["ps0"], writes=[f"GT{g_}"])
                sc.add("dve", lambda e: e.tensor_copy(out=GT[g_][:, 4:8, :],
                                                      in_=ps[1][:].rearrange("p (k c) -> p k c", k=4)),
                       reads=["ps1"], writes=[f"GT{g_}"])

            def c2(m):
                s_ = m % 4
                g_ = m % 2
                for half in range(2):
                    def omm(e, half=half):
                        ins = None
                        for kc in range(8):
                            ins = e.matmul(ps[2 + half][:, :], lhsT=GT[g_][:, kc, :],
                                           rhs=wo[:, kc, half * 512:(half + 1) * 512], start=(kc == 0), stop=(kc == 7))
                        return ins
                    sc.add("pe", omm, reads=[f"GT{g_}", "wo"], writes=[f"ps{2 + half}"])
                    sc.add("dve", lambda e, half=half: e.tensor_tensor(
                        out=xt[s_][:, half * 512:(half + 1) * 512], in0=ps[2 + half][:, :],
                        in1=xt[s_][:, half * 512:(half + 1) * 512], op=ALU.add),
                        reads=[f"ps{2 + half}", f"xt{s_}"], writes=[f"xt{s_}"])

            def c3(m):
                nat = (NT - 1 - m) if rev else m
                s_ = m % 4
                if l == 0:
                    sc.add("sp", lambda e: e.dma_start(out=h1_d[nat * 128:(nat + 1) * 128, :], in_=xt[s_]),
                           reads=[f"xt{s_}"], writes=[f"h1_{nat}"], dma_key=f"st{s_}")
                    if 1 in layers:
                        norm_part(s_)
                else:
                    norm_part(s_, final=True)

            def c4(m):
                nat = (NT - 1 - m) if rev else m
                s_ = m % 4
                if l == 0:
                    if 1 in layers:
                        tr_part(s_, NT - 1 - nat, True, 4)
                else:
                    sc.add("sp", lambda e: e.dma_start(out=out_d[nat * 128:(nat + 1) * 128, :], in_=xt[s_]),
                           reads=[f"xt{s_}"], writes=[f"out_{nat}"], dma_key=f"st{s_}")
            run_pipeline(list(range(NT)), [(c1, 0), (c2, 1), (c3, 2), (c4, 3)])

        for l in layers:
            moba = (l == 0)
            if l == 1 and 0 not in layers:
                raise NotImplementedError
            load_wh(l, heads[0], 0)
            if stop != "A":
                for grp in qk_groups(l, heads[0], 0, moba, 0):
                    grp()
            if stop != "A":
                for grp in vz_groups(l, heads[0], 0):
                    grp()
            for hi, h in enumerate(heads):
                if stop == "A":
                    continue
                par = hi % 2
                fill = []
                vzg = None
                if hi + 1 < len(heads):
                    load_wh(l, heads[hi + 1], (hi + 1) % 2)
                    fill = qk_groups(l, heads[hi + 1], (hi + 1) % 2, moba, 1 - par)
                    vzg = vz_groups(l, heads[hi + 1], (hi + 1) % 2)
                if moba:
                    moba_attn(h, par, fill, vzg)
                else:
                    sb_attn(h, par, fill, vzg)
            sc.barrier()
            if debug and l == layers[-1]:
                sc.add("sp", lambda e: e.dma_start(out=gd_d[:, :, :], in_=G[:, :, :]), reads=[], writes=["gd"], dma_key="stg")
                sc.barrier()
            if stop in (None, "C"):
                phase_c(l, x_d if l == 0 else h1_d)
            sc.barrier()
        if 1 not in layers:
            pass

        fin = Op("sp", None)
        fin.deps = [o for k, o in sc.all_dma_last.items() if k.startswith("st")]
        sc.ops["sp"].append(fin)

        sc.finalize()
        esems = {e: [st.enter_context(nc.semaphore(f"s_{e}{i}")) for i in range(sc.nsem[e])] for e in ENGS}
        dsems = {k: st.enter_context(nc.semaphore(f"d_{k}")) for k in sc.dma_cnt}
        block = st.enter_context(nc.Block())

        @block.tensor
        def _(e):
            sc.emit("pe", e, esems, dsems)

        @block.scalar
        def _(e):
            sc.emit("act", e, esems, dsems)

        @block.vector
        def _(e):
            sc.emit("dve", e, esems, dsems)

        @block.gpsimd
        def _(e):
            sc.emit("pool", e, esems, dsems)

        @block.sync
        def _(e):
            sc.emit("sp", e, esems, dsems)
    return nc


def _consts():
    cbf = np.zeros((128, 512), np.float32)
    p = np.arange(128)
    cbf[p, p] = 1.0
    cbf[p, 128 + (127 - p)] = 1.0
    cbf[:, 256:384] = np.where(p[:, None] > p[None, :], -BIGPEN, 0.0)
    cbf[:, 384:512] = np.where(p[None, :] <= p[:, None], -SBPEN, 0.0)
    cf = np.zeros((128, 512), np.float32)
    for h in range(H):
        for idx in range(32):
            cf[:, h * 32 + idx] = np.float32(SLOPES[h]) * (128.0 * (idx - 28) + p)
    t = np.arange(S)
    augk = np.zeros((32, S), np.float32)
    augk[t // 256, t] = 1.0
    augk[16:18, :] = 1.0
    augq = np.zeros((16, S), np.float32)
    augq[0] = -128.0 * ((t % 512) // 128)
    augq[1] = -(t % 128)
    return cbf, cf, augk, augq


_NC_CACHE = {}


def kernel(x, norm_g, w_in, w_out, final_g):
    x = np.asarray(x, np.float32)
    w_in = np.asarray(w_in, np.float32)
    w_out = np.asarray(w_out, np.float32)
    norm_g = np.asarray(norm_g, np.float32)
    final_g = np.asarray(final_g, np.float32)
    B = x.shape[0]
    wi = w_in.reshape(2, 8, 128, 4, H, DH)
    win = np.ascontiguousarray(wi.transpose(0, 4, 2, 1, 3, 5)).reshape(2, H, 128, 8, 256)
    wout = np.ascontiguousarray(w_out.reshape(2, 8, 128, D).transpose(0, 2, 1, 3))
    gb = np.ascontiguousarray(np.broadcast_to(
        np.concatenate([norm_g, final_g[None, :]], axis=0)[:, None, :], (3, 128, D)))
    cbf, cf, augk, augq = _consts()
    if "nc" not in _NC_CACHE:
        _NC_CACHE["nc"] = build_program()
    nc = _NC_CACHE["nc"]
    in_maps = [{"x": np.ascontiguousarray(x[b]), "win": win, "wout": wout, "gb": gb, "cbf": cbf, "cf": cf,
                "augk": augk, "augq": augq} for b in range(B)]
    res = run_bass_kernel_spmd(nc, in_maps, core_ids=list(range(B)))
    return np.stack([np.asarray(r["out"]) for r in res.results], axis=0).astype(np.float32)
```

```python
import contextlib
import numpy as np
import concourse.bass as bass
import concourse.mybir as mybir
from concourse.bass_utils import run_bass_kernel_spmd

F32 = mybir.dt.float32
BF16 = mybir.dt.bfloat16
AF = mybir.ActivationFunctionType
ALU = mybir.AluOpType
AX = mybir.AxisListType

S = 4096
D = 1024
H = 16
DH = 64
NT = S // 128
EPS = 1e-6
BIGPEN = 65536.0
SBPEN = 32768.0
SLOPES = [2.0 ** (-(h + 1) / 2.0) for h in range(H)]

ENGS = ("pe", "act", "dve", "pool", "sp")
ROT = 8000


class Op:
    __slots__ = ("eng", "fn", "deps", "signal", "seq", "semi", "is_dma", "key", "cnt", "idx")

    def __init__(self, eng, fn, is_dma=False, key=None):
        self.eng = eng
        self.fn = fn
        self.deps = []
        self.signal = False
        self.seq = 0
        self.semi = 0
        self.is_dma = is_dma
        self.key = key
        self.cnt = 0


class Sched:
    def __init__(self):
        self.ops = {e: [] for e in ENGS}
        self.lastw = {}
        self.readers = {}
        self.dma_cnt = {}
        self.all_dma_last = {}

    def add(self, eng, fn, reads=(), writes=(), dma_key=None):
        op = Op(eng, fn, is_dma=dma_key is not None, key=dma_key)
        deps = {}

        def dep(o, kind):
            if o is None or o is op:
                return
            if not o.is_dma and not op.is_dma and o.eng == eng:
                if eng == "pe":
                    return
            deps[id(o)] = o

        rset = []
        wset = list(writes)
        for r in reads:
            if r.startswith("ps"):
                wset.append(r)
            else:
                rset.append(r)
        for r in rset:
            dep(self.lastw.get(r), "raw")
        for w in wset:
            dep(self.lastw.get(w), "raw" if w.startswith("ps") else "waw")
            for o in self.readers.get(w, {}).values():
                dep(o, "war")
        op.deps = list(deps.values())
        for o in op.deps:
            o.signal = True
        for r in rset:
            d = self.readers.setdefault(r, {})
            d[("dma", id(op)) if op.is_dma else eng] = op
        for w in wset:
            self.lastw[w] = op
            self.readers[w] = {}
        if op.is_dma:
            self.dma_cnt[dma_key] = self.dma_cnt.get(dma_key, 0) + 1
            op.cnt = self.dma_cnt[dma_key]
            self.all_dma_last[dma_key] = op
        self.ops[eng].append(op)
        return op

    def barrier(self):
        lasts = []
        for e in ENGS:
            for o in reversed(self.ops[e]):
                if not o.is_dma and o.fn is not None:
                    lasts.append(o)
                    break
        lasts += list(self.all_dma_last.values())
        for e in ENGS:
            op = Op(e, None)
            op.deps = [o for o in lasts if o.is_dma or o.eng != e]
            for o in op.deps:
                o.signal = True
            self.ops[e].append(op)

    def finalize(self):
        self.nsem = {}
        for e in ENGS:
            c = 0
            for o in self.ops[e]:
                if o.signal and not o.is_dma:
                    o.semi = c // ROT
                    o.seq = c % ROT + 1
                    c += 1
            self.nsem[e] = max(1, (c + ROT - 1) // ROT)

    def emit(self, eng, e, esems, dsems):
        waited = {}
        for o in self.ops[eng]:
            for d in o.deps:
                if d.is_dma:
                    k = ("d", d.key)
                    v = 16 * d.cnt
                    sem = dsems[d.key]
                else:
                    k = (d.eng, d.semi)
                    v = d.seq
                    sem = esems[d.eng][d.semi]
                if waited.get(k, 0) >= v:
                    continue
                waited[k] = v
                e.wait_ge(sem, v)
            if o.fn is None:
                continue
            ins = o.fn(e)
            if o.is_dma:
                ins.then_inc(dsems[o.key], 16)
            elif o.signal:
                ins.then_inc(esems[eng][o.semi], 1)


def build_program(layers=(0, 1), debug=False, heads=None, stop=None):
    heads = list(range(H)) if heads is None else list(heads)
    nc = bass.Bass("TRN2", target_bir_lowering=False)
    x_d = nc.dram_tensor("x", [S, D], F32, kind="ExternalInput").ap()
    win_d = nc.dram_tensor("win", [2, H, 128, 8, 256], F32, kind="ExternalInput").ap()
    wout_d = nc.dram_tensor("wout", [2, 128, 8, D], F32, kind="ExternalInput").ap()
    gb_d = nc.dram_tensor("gb", [3, 128, D], F32, kind="ExternalInput").ap()
    cbf_d = nc.dram_tensor("cbf", [128, 512], F32, kind="ExternalInput").ap()
    cf_d = nc.dram_tensor("cf", [128, 512], F32, kind="ExternalInput").ap()
    augk_d = nc.dram_tensor("augk", [32, S], F32, kind="ExternalInput").ap()
    augq_d = nc.dram_tensor("augq", [16, S], F32, kind="ExternalInput").ap()
    out_d = nc.dram_tensor("out", [S, D], F32, kind="ExternalOutput").ap()
    if debug:
        h1_d = nc.dram_tensor("h1", [S, D], F32, kind="ExternalOutput").ap()
        gd_d = nc.dram_tensor("gd", [128, NT, D], BF16, kind="ExternalOutput").ap()
    else:
        h1_d = nc.dram_tensor("h1", [S, D], F32, kind="Internal").ap()

    sc = Sched()
    st = contextlib.ExitStack()
    with st:
        def sb(name, shape, dt):
            return st.enter_context(nc.sbuf_tensor("sb_" + name, shape, dt))

        hnT = sb("hnT", [128, 8, S], BF16)
        G = sb("G", [128, NT, D], BF16)
        cbf = sb("cbf", [128, 512], BF16)
        cf = sb("cf", [128, 512], F32)
        zeros = sb("zeros", [128, 512], F32)
        penpad = sb("penpad", [128, 4, 128], BF16)
        small = sb("small", [128, 64], F32)
        kms = sb("kms", [128, 16], F32)
        kmb = sb("kmb", [128, 16], BF16)
        gsb = sb("gsb", [128, 64], F32)
        m8 = sb("m8", [128, 32], F32)
        rec = sb("rec", [128, 8], F32)
        POOLW = 18600
        pool = sb("pool", [128, POOLW], F32)
        off = [0]

        def carve(words, dt=F32):
            a = off[0]
            off[0] += words
            assert off[0] <= POOLW, off[0]
            v = pool[:, a:a + words]
            if dt is BF16:
                v = v.bitcast(BF16)
            return v

        qkb = [carve(4096, BF16) for _ in range(2)]
        qTs = [b[:, 0:S] for b in qkb]
        kTs = [b[:, S:2 * S] for b in qkb]
        wh = [carve(1024, BF16).rearrange("p (k c) -> p k c", k=8) for _ in range(2)]
        V = carve(NT * 36, BF16).rearrange("p (t c) -> p t c", t=NT)
        sz = carve(NT * 64, F32).rearrange("p (t c) -> p t c", t=NT)
        wk0 = off[0]
        wTm = [carve(256, BF16) for _ in range(4)]
        off[0] = wk0
        abuf = [carve(512, F32) for _ in range(3)]
        incl = [carve(516, F32) for _ in range(4)]
        wbuf = [carve(256, BF16) for _ in range(3)]
        wTs = [carve(256, BF16) for _ in range(3)]
        endB = off[0]
        off[0] = 0
        wo = carve(4096, BF16).rearrange("p (k c) -> p k c", k=8)
        xt = [carve(1024, F32) for _ in range(4)]
        hn = [carve(512, BF16) for _ in range(4)]
        gbc = carve(1024, F32)
        GT = [carve(512, BF16).rearrange("p (k c) -> p k c", k=8) for _ in range(2)]
        junk = carve(512, BF16)
        assert off[0] <= POOLW

        ps = [st.enter_context(nc.psum_tensor(f"ps{i}", [128, 512], F32)) for i in range(8)]

        ident = cbf[:, 0:128]
        Jm = cbf[:, 128:256]
        tripen = cbf[:, 256:384]
        sbpen = cbf[:, 384:512]
        ss = small[:, 16:20]
        lnv = small[:, 20:24]
        rstd = small[:, 24:28]

        sc.add("pool", lambda e: e.dma_start(out=cbf[:], in_=cbf_d[:, :]), writes=["cbf"], dma_key="cst")
        sc.add("sp", lambda e: e.dma_start(out=cf[:], in_=cf_d[:, :]), writes=["cf"], dma_key="cst2")
        sc.add("pool", lambda e: e.memset(zeros[:], 0.0), writes=["zeros"])
        if debug:
            for t in range(NT):
                sc.add("pool", lambda e, t=t: e.memset(G[:, t, :], 0.0), writes=[f"G{t}"])
        sc.add("pool", lambda e: e.memset(penpad[:], 0.0), writes=["penpad"])

        def run_pipeline(items, stages, fillers=(), post=None):
            n = len(items)
            maxoff = max(o for _, o in stages)
            nsteps = n + maxoff
            fillers = list(fillers)
            nf = len(fillers)
            lo, hi = 8, max(9, nsteps - 8)
            fpos = {}
            for fi in range(nf):
                fpos.setdefault(lo + (fi * (hi - lo)) // max(nf, 1), []).append(fillers[fi])
            for i in range(nsteps):
                for fn, offk in stages:
                    j = i - offk
                    if 0 <= j < n:
                        fn(items[j])
                for f in fpos.get(i, ()):
                    f()
                if post:
                    for f in post.get(i - maxoff, ()):
                        f()

        def load_gb(i):
            sc.add("sp", lambda e: e.dma_start(out=gbc, in_=gb_d[i]), writes=["gbc"], dma_key="gbc")

        def hres(t0, n=1):
            return [f"hnT{t}" for t in range(t0, t0 + n)]

        def norm_part(s, final=False):
            sc.add("act", lambda e: e.activation(out=junk, in_=xt[s], func=AF.Square, accum_out=ss[:, s:s + 1]),
                   reads=[f"xt{s}"], writes=["junk", f"ss{s}"])
            sc.add("act", lambda e: e.activation(out=lnv[:, s:s + 1], in_=ss[:, s:s + 1], func=AF.Ln,
                                                 scale=1.0 / D, bias=eps_ap),
                   reads=[f"ss{s}", "eps"], writes=[f"lnv{s}"])
            sc.add("act", lambda e: e.activation(out=rstd[:, s:s + 1], in_=lnv[:, s:s + 1], func=AF.Exp, scale=-0.5),
                   reads=[f"lnv{s}"], writes=[f"rstd{s}"])
            if final:
                sc.add("dve", lambda e: e.scalar_tensor_tensor(out=xt[s], in0=xt[s], scalar=rstd[:, s:s + 1], in1=gbc,
                                                               op0=ALU.mult, op1=ALU.mult),
                       reads=[f"xt{s}", f"rstd{s}", "gbc"], writes=[f"xt{s}"])
            else:
                sc.add("dve", lambda e: e.scalar_tensor_tensor(out=hn[s], in0=xt[s], scalar=rstd[:, s:s + 1], in1=gbc,
                                                               op0=ALU.mult, op1=ALU.mult),
                       reads=[f"xt{s}", f"rstd{s}", "gbc"], writes=[f"hn{s}"])

        def tr_part(s, dst_tile, rev, psb):
            mat = Jm if rev else ident

            def tr(e, half):
                ins = None
                for c in range(4):
                    kc = half * 4 + c
                    ins = e.matmul(ps[psb + half][:, c * 128:(c + 1) * 128], lhsT=hn[s][:, kc * 128:(kc + 1) * 128],
                                   rhs=mat, start=True, stop=True)
                return ins
            for half in range(2):
                sc.add("pe", lambda e, half=half: tr(e, half), reads=[f"hn{s}", "cbf"], writes=[f"ps{psb + half}"])
            cols = slice(dst_tile * 128, (dst_tile + 1) * 128)
            sc.add("act", lambda e: e.activation(out=hnT[:, 0:4, cols], in_=ps[psb][:].rearrange("p (k c) -> p k c", k=4),
                                                 func=AF.Copy),
                   reads=[f"ps{psb}"], writes=hres(dst_tile))
            sc.add("dve", lambda e: e.tensor_copy(out=hnT[:, 4:8, cols],
                                                  in_=ps[psb + 1][:].rearrange("p (k c) -> p k c", k=4)),
                   reads=[f"ps{psb + 1}"], writes=hres(dst_tile))

        eps_ap = small[:, 8:9]
        ones_col = small[:, 9:10]
        sc.add("dve", lambda e: e.memset(eps_ap, EPS), writes=["eps"])
        sc.add("dve", lambda e: e.memset(ones_col, 1.0), writes=["ones"])

        if 0 in layers:
            load_gb(0)
            def a1(t):
                s_ = t % 4
                sc.add("sp", lambda e: e.dma_start(out=xt[s_], in_=x_d[t * 128:(t + 1) * 128, :]),
                       writes=[f"xt{s_}"], dma_key=f"xt{s_}")
                norm_part(s_)

            def a2(t):
                tr_part(t % 4, t, False, (t % 2) * 2)
            run_pipeline(list(range(NT)), [(a1, 0), (a2, 1)])
            sc.barrier()
            for par in range(2):
                sc.add("pool", lambda e, par=par: e.dma_start(out=kTs[par][64:96, :], in_=augk_d[:, :],
                                                              max_dma_last_dim=4096),
                       writes=[f"kTaug{par}"], dma_key=f"cstk{par}")
                sc.add("pool", lambda e, par=par: e.dma_start(out=qTs[par][80:96, :], in_=augq_d[:, :],
                                                              max_dma_last_dim=4096),
                       writes=[f"qTaug{par}"], dma_key=f"cstq{par}")
            sc.add("dve", lambda e: e.memset(V[:, :, 64:65], 1.0), writes=["Vones"])

        def load_wh(l, h, sl):
            sc.add("pool", lambda e: e.dma_start(out=wh[sl], in_=win_d[l, h], max_dma_last_dim=4096),
                   writes=[f"wh{sl}"], dma_key=f"wh{sl}")

        def qk_groups(l, h, sl, moba, par):
            w = wh[sl]
            qT, kT = qTs[par], kTs[par]
            qscale = (0.125 / SLOPES[h]) if moba else 0.125
            groups = []
            for which in range(2):
                for tt in range(8):
                    def grp(which=which, tt=tt):
                        bank = 6 + (tt % 2)

                        def mm(e):
                            ins = None
                            for kc in range(8):
                                ins = e.matmul(ps[bank][0:64, :], lhsT=w[:, kc, which * 64:(which + 1) * 64],
                                               rhs=hnT[:, kc, tt * 512:(tt + 1) * 512], start=(kc == 0),
                                               stop=(kc == 7))
                            return ins
                        sc.add("pe", mm, reads=[f"wh{sl}"] + hres(tt * 4, 4), writes=[f"ps{bank}"])
                        cols = slice(tt * 512, (tt + 1) * 512)
                        if which == 0:
                            if moba:
                                sc.add("dve", lambda e: e.tensor_scalar(out=qT[0:64, cols], in0=ps[bank][0:64, :],
                                                                        scalar1=qscale, scalar2=None, op0=ALU.mult),
                                       reads=[f"ps{bank}"], writes=[f"qT{par}"])
                            else:
                                sc.add("act", lambda e: e.activation(out=qT[0:64, cols], in_=ps[bank][0:64, :],
                                                                     func=AF.Copy, scale=qscale),
                                       reads=[f"ps{bank}"], writes=[f"qT{par}"])
                        else:
                            if moba:
                                sc.add("dve", lambda e: e.tensor_reduce(
                                    out=kms[0:64, 2 * tt:2 * tt + 2],
                                    in_=ps[bank][0:64, :].rearrange("p (a b) -> p a b", a=2), axis=AX.X, op=ALU.add),
                                    reads=[f"ps{bank}"], writes=["kms"])
                                sc.add("dve", lambda e: e.tensor_copy(out=kT[0:64, cols], in_=ps[bank][0:64, :]),
                                       reads=[f"ps{bank}"], writes=[f"kT{par}"])
                            else:
                                sc.add("act", lambda e: e.activation(out=kT[0:64, cols], in_=ps[bank][0:64, :],
                                                                     func=AF.Copy),
                                       reads=[f"ps{bank}"], writes=[f"kT{par}"])
                    groups.append(grp)
            if moba:
                def g0():
                    sc.add("dve", lambda e: e.tensor_scalar(out=kmb[0:64, :], in0=kms[0:64, :], scalar1=1.0 / 256.0,
                                                            scalar2=None, op0=ALU.mult),
                           reads=["kms"], writes=["kmb"])
                groups.append(g0)
                for g in range(8):
                    def ggrp(g=g):
                        def gmm(e):
                            ins = None
                            for i in range(4):
                                t = 4 * g + i
                                ins = e.matmul(ps[2][:, i * 16:(i + 1) * 16], lhsT=qT[0:64, t * 128:(t + 1) * 128],
                                               rhs=kmb[0:64, :], start=True, stop=True)
                            return ins
                        sc.add("pe", gmm, reads=[f"qT{par}", "kmb"], writes=["ps2"])
                        for i in range(4):
                            t = 4 * g + i
                            cur = t // 2
                            gs = gsb[:, i * 16:(i + 1) * 16]
                            sc.add("dve", lambda e, gs=gs: e.memset(gs, -3.0e38), writes=[f"gsb{i}"])
                            if cur > 0:
                                sc.add("dve", lambda e, i=i, cur=cur, gs=gs: e.tensor_copy(
                                    out=gs[:, 0:cur], in_=ps[2][:, i * 16:i * 16 + cur]),
                                    reads=["ps2"], writes=[f"gsb{i}"])
                            sc.add("dve", lambda e, i=i, gs=gs: e.max(out=m8[:, i * 8:(i + 1) * 8], in_=gs),
                                   reads=[f"gsb{i}"], writes=[f"m8{i}"])
                            sc.add("dve", lambda e, i=i, gs=gs: e.tensor_scalar(
                                out=penpad[:, i, 64:80], in0=gs, scalar1=m8[:, i * 8 + 2:i * 8 + 3],
                                scalar2=-BIGPEN, op0=ALU.is_lt, op1=ALU.mult),
                                reads=[f"gsb{i}", f"m8{i}"], writes=[f"penpad{i}"])
                            sc.add("dve", lambda e, i=i, cur=cur: e.memset(penpad[:, i, 64 + cur:65 + cur], 0.0),
                                   reads=[f"penpad{i}"], writes=[f"penpad{i}"])

                        def pmm(e):
                            ins = None
                            for i in range(4):
                                ins = e.matmul(ps[3][:, i * 128:(i + 1) * 128], lhsT=penpad[:, i, :], rhs=ident,
                                               start=True, stop=True)
                            return ins
                        sc.add("pe", pmm, reads=[f"penpad{i}" for i in range(4)] + ["penpad", "cbf"], writes=["ps3"])
                        sc.add("act", lambda e: e.activation(out=qT[64:80, g * 512:(g + 1) * 512], in_=ps[3][64:80, :],
                                                             func=AF.Copy),
                               reads=["ps3"], writes=[f"qT{par}"])
                    groups.append(ggrp)
            return groups

        def vz_groups(l, h, sl, moba=True):
            w = wh[sl]
            groups = []
            for g in range(8):
                def grp(g=g):
                    bank = 6 + (g % 2)

                    def mmvz(e):
                        ins = None
                        for i in range(4):
                            t = 4 * g + i
                            for kc in range(8):
                                ins = e.matmul(ps[bank][:, i * 128:(i + 1) * 128],
                                               lhsT=hnT[:, kc, t * 128:(t + 1) * 128],
                                               rhs=w[:, kc, 128:256], start=(kc == 0), stop=(kc == 7))
                        return ins
                    sc.add("pe", mmvz, reads=[f"wh{sl}"] + hres(4 * g, 4), writes=[f"ps{bank}"])
                    pv = ps[bank][:].rearrange("p (i c) -> p i c", i=4)
                    sc.add("dve", lambda e: e.tensor_copy(out=V[:, 4 * g:4 * g + 4, 0:64], in_=pv[:, :, 0:64]),
                           reads=[f"ps{bank}"], writes=[f"V{4 * g + i}" for i in range(4)])
                    szr = [f"sz{4 * g + i}" for i in range(4)]
                    if moba:
                        sc.add("act", lambda e: e.activation(out=sz[:, 4 * g:4 * g + 4, :], in_=pv[:, :, 64:128],
                                                             func=AF.Silu),
                               reads=[f"ps{bank}"], writes=szr)
                    else:
                        sc.add("act", lambda e: e.activation(out=sz[:, 4 * g:4 * g + 4, :], in_=pv[:, :, 64:128],
                                                             func=AF.Sigmoid),
                               reads=[f"ps{bank}"], writes=szr)
                        sc.add("dve", lambda e: e.tensor_tensor(out=sz[:, 4 * g:4 * g + 4, :], in0=pv[:, :, 64:128],
                                                                in1=sz[:, 4 * g:4 * g + 4, :], op=ALU.mult),
                               reads=[f"ps{bank}"] + szr, writes=szr)
                groups.append(grp)
            return groups

        def moba_attn(h, par, fillers, vzg):
            slope = SLOPES[h]
            qT, kT = qTs[par], kTs[par]
            items = []
            post = {}
            for qt in range(7, -1, -1):
                nk = 4 * qt + 4
                for kt in range(nk):
                    items.append(dict(qt=qt, kt=kt, nk=nk, idx=len(items)))
                if vzg:
                    post[len(items) - 1] = [vzg[qt]]

            def stA(it):
                qt, kt, idx = it["qt"], it["kt"], it["idx"]
                j = kt - 4 * qt
                c0 = 128 * max(j, 0)
                sbk = idx % 2
                w_i = idx % 4

                def smm(e):
                    ins = e.matmul(ps[sbk][:, c0:512], lhsT=kT[0:82, kt * 128:(kt + 1) * 128],
                                   rhs=qT[0:82, qt * 512 + c0:(qt + 1) * 512], start=True, stop=(j < 0))
                    if j >= 0:
                        ins = e.matmul(ps[sbk][:, c0:c0 + 128], lhsT=ident, rhs=tripen, start=False, stop=True)
                    return ins
                sc.add("pe", smm, reads=[f"qT{par}", f"kT{par}", f"qTaug{par}", f"kTaug{par}", "cbf"],
                       writes=[f"ps{sbk}"])
                bcol = h * 32 + (j + 28)
                sc.add("act", lambda e: e.activation(out=wTm[w_i][:, c0:512], in_=ps[sbk][:, c0:512], func=AF.Exp,
                                                     scale=slope, bias=cf[:, bcol:bcol + 1]),
                       reads=[f"ps{sbk}", "cf"], writes=[f"wTm{w_i}"])

            def stB(it):
                qt, kt, nk, idx = it["qt"], it["kt"], it["nk"], it["idx"]
                j = kt - 4 * qt
                w_i = idx % 4
                ab = 4 + (qt % 2)

                def pvmm(e):
                    ins = None
                    for i in range(max(j, 0), 4):
                        ins = e.matmul(ps[ab][:, i * 128:i * 128 + 65], lhsT=wTm[w_i][:, i * 128:(i + 1) * 128],
                                       rhs=V[:, kt, 0:65], start=(kt == 0 and i == 0), stop=(kt == nk - 1),
                                       skip_group_check=True)
                    return ins
                sc.add("pe", pvmm, reads=[f"wTm{w_i}", f"V{kt}", "Vones"], writes=[f"ps{ab}"])
                if kt == nk - 1:
                    accv = ps[ab][:].rearrange("p (i c) -> p i c", i=4)
                    r0 = 4 * (qt % 2)
                    sc.add("dve", lambda e: e.reciprocal(
                        out=rec[:, r0:r0 + 4].rearrange("p (i o) -> p i o", o=1), in_=accv[:, :, 64:65]),
                        reads=[f"ps{ab}"], writes=[f"rec{qt % 2}"])
                    for i in range(4):
                        t = 4 * qt + i
                        sc.add("dve", lambda e, i=i, t=t: e.scalar_tensor_tensor(
                            out=G[:, t, h * 64:(h + 1) * 64], in0=accv[:, i, 0:64], scalar=rec[:, r0 + i:r0 + i + 1],
                            in1=sz[:, t, :], op0=ALU.mult, op1=ALU.mult),
                            reads=[f"ps{ab}", f"rec{qt % 2}", f"sz{t}"], writes=[f"G{t}"])
            run_pipeline(items, [(stA, 0), (stB, 2)], fillers, post)

        def sb_attn(h, par, fillers, vzg):
            qT, kT = qTs[par], kTs[par]
            items = []
            post = {}
            for iq in range(NT):
                nch = (NT - iq + 3) // 4
                for c in range(nch):
                    k0 = 128 * iq + 512 * c
                    N = min(512, S - k0)
                    items.append(dict(iq=iq, c=c, nch=nch, k0=k0, N=N, idx=len(items)))
                if vzg and iq % 4 == 3:
                    post[len(items) - 1] = [vzg[iq // 4]]

            def stA(it):
                iq, c, k0, N, idx = it["iq"], it["c"], it["k0"], it["N"], it["idx"]
                y = idx % 3
                sbk = idx % 2

                def smm(e):
                    ins = e.matmul(ps[sbk][:, 0:N], lhsT=qT[0:64, iq * 128:(iq + 1) * 128], rhs=kT[0:64, k0:k0 + N],
                                   start=True, stop=(c > 0))
                    if c == 0:
                        ins = e.matmul(ps[sbk][:, 0:128], lhsT=ident, rhs=sbpen, start=False, stop=True)
                    return ins
                sc.add("pe", smm, reads=[f"qT{par}", f"kT{par}", "cbf"], writes=[f"ps{sbk}"])
                sc.add("act", lambda e: e.activation(out=abuf[y][:, 0:N], in_=ps[sbk][:, 0:N], func=AF.Sigmoid,
                                                     scale=-1.0),
                       reads=[f"ps{sbk}"], writes=[f"abuf{y}"])

            def stA1(it):
                c, N, idx = it["c"], it["N"], it["idx"]
                y = idx % 3
                yi = idx % 4
                py = (idx - 1) % 4
                init = ones_col if c == 0 else incl[py][:, 512:513]
                rd = ["ones"] if c == 0 else [f"incl{py}"]
                sc.add("dve", lambda e: e.tensor_tensor_scan(
                    out=incl[yi][:, 1:N + 1], data0=abuf[y][:, 0:N], data1=zeros[:, 0:N], initial=init,
                    op0=ALU.mult, op1=ALU.add),
                    reads=[f"abuf{y}", "zeros"] + rd, writes=[f"incl{yi}"])

            def stB1(it):
                c, N, idx = it["c"], it["N"], it["idx"]
                y = idx % 3
                yi = idx % 4
                py = (idx - 1) % 4
                init = ones_col if c == 0 else incl[py][:, 512:513]
                rd = ["ones"] if c == 0 else [f"incl{py}"]
                sc.add("dve", lambda e: e.tensor_tensor(out=wbuf[y][:, 1:N], in0=incl[yi][:, 1:N],
                                                        in1=incl[yi][:, 2:N + 1], op=ALU.subtract),
                       reads=[f"incl{yi}"], writes=[f"wbuf{y}"])
                sc.add("act", lambda e: e.activation(out=wbuf[y][:, 0:1], in_=incl[yi][:, 1:2], func=AF.Identity,
                                                     scale=-1.0, bias=init),
                       reads=[f"incl{yi}"] + rd, writes=[f"wbufc{y}"])

            def stT(it):
                N, idx = it["N"], it["idx"]
                y = idx % 3
                tbk = 2 + (idx % 2)
                nb = N // 128

                def tmm(e):
                    ins = None
                    for b in range(nb):
                        ins = e.matmul(ps[tbk][:, b * 128:(b + 1) * 128], lhsT=wbuf[y][:, b * 128:(b + 1) * 128],
                                       rhs=ident, start=True, stop=True)
                    return ins
                sc.add("pe", tmm, reads=[f"wbuf{y}", f"wbufc{y}", "cbf"], writes=[f"ps{tbk}"])

            def stE(it):
                N, idx = it["N"], it["idx"]
                y = idx % 3
                tbk = 2 + (idx % 2)
                sc.add("act", lambda e: e.activation(out=wTs[y][:, 0:N], in_=ps[tbk][:, 0:N], func=AF.Copy),
                       reads=[f"ps{tbk}"], writes=[f"wTs{y}"])

            def stC(it):
                iq, c, nch, N, idx = it["iq"], it["c"], it["nch"], it["N"], it["idx"]
                y = idx % 3
                nb = N // 128
                ab = 4 + (iq % 2)

                def pvmm(e):
                    ins = None
                    for b in range(nb):
                        ins = e.matmul(ps[ab][:, 0:64], lhsT=wTs[y][:, b * 128:(b + 1) * 128],
                                       rhs=V[:, iq + 4 * c + b, 0:64], start=(c == 0 and b == 0),
                                       stop=(c == nch - 1 and b == nb - 1))
                    return ins
                sc.add("pe", pvmm, reads=[f"wTs{y}"] + [f"V{iq + 4 * c + b}" for b in range(nb)], writes=[f"ps{ab}"])
                if c == nch - 1:
                    sc.add("dve", lambda e: e.tensor_tensor(out=G[:, iq, h * 64:(h + 1) * 64], in0=ps[ab][:, 0:64],
                                                            in1=sz[:, iq, :], op=ALU.mult),
                           reads=[f"ps{ab}", f"sz{iq}"], writes=[f"G{iq}"])
            run_pipeline(items, [(stA1, 1), (stE, 4), (stA, 0), (stC, 5), (stB1, 2), (stT, 3)], fillers, post)

        def phase_c(l, hsrc_d):
            sc.add("pool", lambda e: e.dma_start(out=wo, in_=wout_d[l], max_dma_last_dim=8192),
                   writes=["wo"], dma_key="wo")
            load_gb(l + 1)
            rev = (l == 1)
            mat = Jm if rev else ident

            def c1(m):
                nat = (NT - 1 - m) if rev else m
                s_ = m % 4
                g_ = m % 2
                sc.add("sp", lambda e: e.dma_start(out=xt[s_], in_=hsrc_d[nat * 128:(nat + 1) * 128, :]),
                       writes=[f"xt{s_}"], dma_key=f"xt{s_}")

                def tr(e, half):
                    ins = None
                    for c in range(4):
                        kc = half * 4 + c
                        ins = e.matmul(ps[half][:, c * 128:(c + 1) * 128], lhsT=G[:, m, kc * 128:(kc + 1) * 128],
                                       rhs=mat, start=True, stop=True)
                    return ins
                for half in range(2):
                    sc.add("pe", lambda e, half=half: tr(e, half), reads=[f"G{m}", "cbf"], writes=[f"ps{half}"])
                sc.add("act", lambda e: e.activation(out=GT[g_][:, 0:4, :],
                                                     in_=ps[0][:].rearrange("p (k c) -> p k c", k=4), func=AF.Copy),
                       reads=["ps0"], writes=[f"GT{g_}"])
                sc.add("dve", lambda e: e.tensor_copy(out=GT[g_][:, 4:8, :],
                                                      in_=ps[1][:].rearrange("p (k c) -> p k c", k=4)),
                       reads=["ps1"], writes=[f"GT{g_}"])

            def c2(m):
                s_ = m % 4
                g_ = m % 2
                for half in range(2):
                    def omm(e, half=half):
                        ins = None
                        for kc in range(8):
                            ins = e.matmul(ps[2 + half][:, :], lhsT=GT[g_][:, kc, :],
                                           rhs=wo[:, kc, half * 512:(half + 1) * 512], start=(kc == 0), stop=(kc == 7))
                        return ins
                    sc.add("pe", omm, reads=[f"GT{g_}", "wo"], writes=[f"ps{2 + half}"])
                    sc.add("dve", lambda e, half=half: e.tensor_tensor(
                        out=xt[s_][:, half * 512:(half + 1) * 512], in0=ps[2 + half][:, :],
                        in1=xt[s_][:, half * 512:(half + 1) * 512], op=ALU.add),
                        reads=[f"ps{2 + half}", f"xt{s_}"], writes=[f"xt{s_}"])

            def c3(m):
                nat = (NT - 1 - m) if rev else m
                s_ = m % 4
                if l == 0:
                    sc.add("sp", lambda e: e.dma_start(out=h1_d[nat * 128:(nat + 1) * 128, :], in_=xt[s_]),
                           reads=[f"xt{s_}"], writes=[f"h1_{nat}"], dma_key=f"st{s_}")
                    if 1 in layers:
                        norm_part(s_)
                else:
                    norm_part(s_, final=True)

            def c4(m):
                nat = (NT - 1 - m) if rev else m
                s_ = m % 4
                if l == 0:
                    if 1 in layers:
                        tr_part(s_, NT - 1 - nat, True, 4)
                else:
                    sc.add("sp", lambda e: e.dma_start(out=out_d[nat * 128:(nat + 1) * 128, :], in_=xt[s_]),
                           reads=[f"xt{s_}"], writes=[f"out_{nat}"], dma_key=f"st{s_}")
            run_pipeline(list(range(NT)), [(c1, 0), (c2, 1), (c3, 2), (c4, 3)])

        for l in layers:
            moba = (l == 0)
            if l == 1 and 0 not in layers:
                raise NotImplementedError
            load_wh(l, heads[0], 0)
            if stop != "A":
                for grp in qk_groups(l, heads[0], 0, moba, 0):
                    grp()
            if stop != "A":
                for grp in vz_groups(l, heads[0], 0, moba):
                    grp()
            for hi, h in enumerate(heads):
                if stop == "A":
                    continue
                par = hi % 2
                fill = []
                vzg = None
                if hi + 1 < len(heads):
                    load_wh(l, heads[hi + 1], (hi + 1) % 2)
                    fill = qk_groups(l, heads[hi + 1], (hi + 1) % 2, moba, 1 - par)
                    vzg = vz_groups(l, heads[hi + 1], (hi + 1) % 2, moba)
                if moba:
                    moba_attn(h, par, fill, vzg)
                else:
                    sb_attn(h, par, fill, vzg)
            sc.barrier()
            if debug and l == layers[-1]:
                sc.add("sp", lambda e: e.dma_start(out=gd_d[:, :, :], in_=G[:, :, :]), reads=[], writes=["gd"], dma_key="stg")
                sc.barrier()
            if stop in (None, "C"):
                phase_c(l, x_d if l == 0 else h1_d)
            sc.barrier()
        if 1 not in layers:
            pass

        fin = Op("sp", None)
        fin.deps = [o for k, o in sc.all_dma_last.items() if k.startswith("st")]
        sc.ops["sp"].append(fin)

        sc.finalize()
        esems = {e: [st.enter_context(nc.semaphore(f"s_{e}{i}")) for i in range(sc.nsem[e])] for e in ENGS}
        dsems = {k: st.enter_context(nc.semaphore(f"d_{k}")) for k in sc.dma_cnt}
        block = st.enter_context(nc.Block())

        @block.tensor
        def _(e):
            sc.emit("pe", e, esems, dsems)

        @block.scalar
        def _(e):
            sc.emit("act", e, esems, dsems)

        @block.vector
        def _(e):
            sc.emit("dve", e, esems, dsems)

        @block.gpsimd
        def _(e):
            sc.emit("pool", e, esems, dsems)

        @block.sync
        def _(e):
            sc.emit("sp", e, esems, dsems)
    return nc


def _consts():
    cbf = np.zeros((128, 512), np.float32)
    p = np.arange(128)
    cbf[p, p] = 1.0
    cbf[p, 128 + (127 - p)] = 1.0
    cbf[:, 256:384] = np.where(p[:, None] > p[None, :], -BIGPEN, 0.0)
    cbf[:, 384:512] = np.where(p[None, :] <= p[:, None], -SBPEN, 0.0)
    cf = np.zeros((128, 512), np.float32)
    for h in range(H):
        for idx in range(32):
            cf[:, h * 32 + idx] = np.float32(SLOPES[h]) * (128.0 * (idx - 28) + p)
    t = np.arange(S)
    augk = np.zeros((32, S), np.float32)
    augk[t // 256, t] = 1.0
    augk[16:18, :] = 1.0
    augq = np.zeros((16, S), np.float32)
    augq[0] = -128.0 * ((t % 512) // 128)
    augq[1] = -(t % 128)
    return cbf, cf, augk, augq


_NC_CACHE = {}


def kernel(x, norm_g, w_in, w_out, final_g):
    x = np.asarray(x, np.float32)
    w_in = np.asarray(w_in, np.float32)
    w_out = np.asarray(w_out, np.float32)
    norm_g = np.asarray(norm_g, np.float32)
    final_g = np.asarray(final_g, np.float32)
    B = x.shape[0]
    wi = w_in.reshape(2, 8, 128, 4, H, DH)
    win = np.ascontiguousarray(wi.transpose(0, 4, 2, 1, 3, 5)).reshape(2, H, 128, 8, 256)
    wout = np.ascontiguousarray(w_out.reshape(2, 8, 128, D).transpose(0, 2, 1, 3))
    gb = np.ascontiguousarray(np.broadcast_to(
        np.concatenate([norm_g, final_g[None, :]], axis=0)[:, None, :], (3, 128, D)))
    cbf, cf, augk, augq = _consts()
    if "nc" not in _NC_CACHE:
        _NC_CACHE["nc"] = build_program()
    nc = _NC_CACHE["nc"]
    in_maps = [{"x": np.ascontiguousarray(x[b]), "win": win, "wout": wout, "gb": gb, "cbf": cbf, "cf": cf,
                "augk": augk, "augq": augq} for b in range(B)]
    res = run_bass_kernel_spmd(nc, in_maps, core_ids=list(range(B)))
    return np.stack([np.asarray(r["out"]) for r in res.results], axis=0).astype(np.float32)
```
